# Optimizing a Trainium2 kernel written in Bass

```python
import math
import jax, jax.numpy as jnp
from jax import lax
import numpy as np

D_MODEL = 2048
BATCH = 4
SEQ = 4096
DEPTH = 1

HEAD_DIM = 128
N_NSA_HEADS = 8
N_NSA_KV = 2
NSA_GROUP = N_NSA_HEADS // N_NSA_KV
N_SB_HEADS = 8
CMP_BLOCK = 32
CMP_STRIDE = 16
CMP_HIDDEN = 256
SLC_BLOCK = 64
N_SELECT = 16
WINDOW = 512
Q_BLOCK = 128
SLC_Q_CHUNK = 64
D_FF = 5632
ROPE_THETA = 10000.0
EPS = 1e-6
NEG_INF = -1e30
FORCE_SCORE = 1e9

kernel_name = "hybrid_nsa_stickbreaking_macaron"


def rms_norm(x, g):
    xf = x.astype(jnp.float32)
    y = xf * lax.rsqrt(jnp.mean(xf * xf, axis=-1, keepdims=True) + EPS)
    return (y * g.astype(jnp.float32)).astype(x.dtype)


def apply_rope(x, cos, sin):
    x1, x2 = jnp.split(x, 2, axis=-1)
    return jnp.concatenate([x1 * cos - x2 * sin, x2 * cos + x1 * sin], axis=-1)


def swiglu(x, w_gate, w_up, w_down):
    return (jax.nn.silu(x @ w_gate) * (x @ w_up)) @ w_down


def compress_blocks(t, pos, w1, w2):
    b, s, g, d = t.shape
    n_cmp = (s - CMP_BLOCK) // CMP_STRIDE + 1
    idx = jnp.arange(n_cmp)[:, None] * CMP_STRIDE + jnp.arange(CMP_BLOCK)[None, :]
    blocks = t[:, idx] + pos[None, None, :, None, :]
    blocks = blocks.transpose(0, 1, 3, 2, 4).reshape(b, n_cmp, g, CMP_BLOCK * d)
    return jax.nn.gelu(blocks @ w1) @ w2


def nsa_attention(q, kc, vc, k_slc, v_slc, k_win, v_win, gates):
    b, s, h, d = q.shape
    g, r = N_NSA_KV, NSA_GROUP
    scale = d ** -0.5
    t = jnp.arange(s)
    qg = q.reshape(b, s, g, r, d).transpose(0, 2, 3, 1, 4)

    n_cmp = kc.shape[1]
    sc = jnp.einsum('bgrtd,bngd->bgrtn', qg, kc, preferred_element_type=jnp.float32) * scale
    blk_end = jnp.arange(n_cmp) * CMP_STRIDE + CMP_BLOCK - 1
    cvalid = blk_end[None, :] <= t[:, None]
    pc = jax.nn.softmax(jnp.where(cvalid, sc, NEG_INF), axis=-1)
    pc = jnp.where(cvalid, pc, 0.0)
    o_cmp = jnp.einsum('bgrtn,bngd->bgrtd', pc.astype(vc.dtype), vc)

    n_slc = s // SLC_BLOCK
    ratio = SLC_BLOCK // CMP_STRIDE
    span = CMP_BLOCK // CMP_STRIDE
    imp = pc.sum(axis=2)
    pad_front = span - 1
    pad_back = max(ratio * n_slc + span - 1 - (n_cmp + pad_front), 0)
    imp = jnp.pad(imp, ((0, 0), (0, 0), (0, 0), (pad_front, pad_back)))
    p_slc = imp[..., 0:ratio * n_slc:ratio]
    for off in range(1, ratio + span - 1):
        p_slc = p_slc + imp[..., off:off + ratio * n_slc:ratio]
    blk = jnp.arange(n_slc)
    cur = t // SLC_BLOCK
    forced = (blk[None, :] == 0) | (blk[None, :] == cur[:, None]) | (blk[None, :] == cur[:, None] - 1)
    causal_ok = blk[None, :] * SLC_BLOCK <= t[:, None]
    score = jnp.where(causal_ok, jnp.where(forced, FORCE_SCORE, p_slc), NEG_INF)
    n_top = min(N_SELECT, n_slc)
    _, sel = lax.top_k(score, n_top)

    kb = k_slc.reshape(b, n_slc, SLC_BLOCK, g, d).transpose(0, 3, 1, 2, 4)
    vb = v_slc.reshape(b, n_slc, SLC_BLOCK, g, d).transpose(0, 3, 1, 2, 4)
    nc = s // SLC_Q_CHUNK
    q_c = qg.reshape(b, g, r, nc, SLC_Q_CHUNK, d).transpose(3, 0, 1, 2, 4, 5)
    sel_c = sel.reshape(b, g, nc, SLC_Q_CHUNK, n_top).transpose(2, 0, 1, 3, 4)
    t_c = t.reshape(nc, SLC_Q_CHUNK)
    bi = jnp.arange(b)[:, None, None, None]
    gi = jnp.arange(g)[None, :, None, None]

    def slc_chunk(args):
        qb, ib, tb = args
        kg = kb[bi, gi, ib]
        vg = vb[bi, gi, ib]
        sc_ = jnp.einsum('bgrqd,bgqnld->bgrqnl', qb, kg, preferred_element_type=jnp.float32) * scale
        kpos = ib[..., None] * SLC_BLOCK + jnp.arange(SLC_BLOCK)
        mask = (kpos <= tb[None, None, :, None, None])[:, :, None]
        sc_ = jnp.where(mask, sc_, NEG_INF).reshape(b, g, r, SLC_Q_CHUNK, n_top * SLC_BLOCK)
        p = jax.nn.softmax(sc_, axis=-1).reshape(b, g, r, SLC_Q_CHUNK, n_top, SLC_BLOCK)
        return jnp.einsum('bgrqnl,bgqnld->bgrqd', p.astype(vg.dtype), vg)

    o_slc = lax.map(slc_chunk, (q_c, sel_c, t_c))
    o_slc = o_slc.transpose(1, 2, 3, 0, 4, 5).reshape(b, g, r, s, d)

    nq = s // Q_BLOCK
    span_w = WINDOW + Q_BLOCK
    widx = jnp.arange(nq)[:, None] * Q_BLOCK + jnp.arange(span_w)[None, :]
    kpad = jnp.pad(k_win, ((0, 0), (WINDOW, 0), (0, 0), (0, 0)))
    vpad = jnp.pad(v_win, ((0, 0), (WINDOW, 0), (0, 0), (0, 0)))
    kw = kpad[:, widx].transpose(0, 3, 1, 2, 4)
    vw = vpad[:, widx].transpose(0, 3, 1, 2, 4)
    qw = qg.reshape(b, g, r, nq, Q_BLOCK, d)
    sw = jnp.einsum('bgriqd,bgikd->bgriqk', qw, kw, preferred_element_type=jnp.float32) * scale
    kpos = widx - WINDOW
    diff = t.reshape(nq, Q_BLOCK)[:, :, None] - kpos[:, None, :]
    wmask = (kpos[:, None, :] >= 0) & (diff >= 0) & (diff < WINDOW)
    pw = jax.nn.softmax(jnp.where(wmask, sw, NEG_INF), axis=-1)
    o_win = jnp.einsum('bgriqk,bgikd->bgriqd', pw.astype(vw.dtype), vw).reshape(b, g, r, s, d)

    gt = jax.nn.sigmoid(gates.astype(jnp.float32)).astype(q.dtype)
    gt = gt.reshape(b, s, g, r, 3).transpose(0, 2, 3, 1, 4)
    o = gt[..., 0:1] * o_cmp + gt[..., 1:2] * o_slc + gt[..., 2:3] * o_win
    return o.transpose(0, 3, 1, 2, 4).reshape(b, s, h, d)


def stick_breaking_attention(q, k, v):
    b, s, h, d = q.shape
    scale = d ** -0.5
    qh, kh, vh = (a.transpose(0, 2, 1, 3) for a in (q, k, v))
    nq = s // Q_BLOCK
    q_c = qh.reshape(b, h, nq, Q_BLOCK, d).transpose(2, 0, 1, 3, 4)
    t_c = jnp.arange(s).reshape(nq, Q_BLOCK)
    kpos = jnp.arange(s)

    def sb_block(args):
        qb, tb = args
        z = jnp.einsum('bhqd,bhkd->bhqk', qb, kh, preferred_element_type=jnp.float32) * scale
        mask = kpos[None, :] < tb[:, None]
        log_keep = jnp.where(mask, jax.nn.log_sigmoid(-z), 0.0)
        after = lax.cumsum(log_keep, axis=3, reverse=True) - log_keep
        a = jnp.where(mask, jnp.exp(jax.nn.log_sigmoid(z) + after), 0.0)
        return jnp.einsum('bhqk,bhkd->bhqd', a.astype(vh.dtype), vh)

    o = lax.map(sb_block, (q_c, t_c))
    return o.transpose(1, 0, 3, 2, 4).reshape(b, s, h, d)


def setup_inputs(seed: int = 0) -> dict:
    key = jax.random.key(seed)
    ks = jax.random.split(key, 32)
    f32 = jnp.float32
    L = DEPTH
    hd = HEAD_DIM
    kv = N_NSA_KV * hd
    in_cols = N_NSA_HEADS * hd + 6 * kv + N_NSA_HEADS * 3 + 3 * N_SB_HEADS * hd
    mix = (N_NSA_HEADS + N_SB_HEADS) * hd

    def nrm(k, shape, fan_in):
        return jax.random.normal(k, shape, f32) * (fan_in ** -0.5)

    def gain(k, shape):
        return 1.0 + 0.1 * jax.random.normal(k, shape, f32)

    return {
        "x": jax.random.normal(ks[0], (BATCH, SEQ, D_MODEL), f32),
        "positions": jnp.broadcast_to(jnp.arange(SEQ, dtype=jnp.int32)[None, :], (BATCH, SEQ)),
        "ffn1_norm": gain(ks[1], (L, D_MODEL)),
        "ffn1_w_gate": nrm(ks[2], (L, D_MODEL, D_FF), D_MODEL),
        "ffn1_w_up": nrm(ks[3], (L, D_MODEL, D_FF), D_MODEL),
        "ffn1_w_down": nrm(ks[4], (L, D_FF, D_MODEL), D_FF),
        "mix_norm": gain(ks[5], (L, D_MODEL)),
        "w_in": nrm(ks[6], (L, D_MODEL, in_cols), D_MODEL),
        "nsa_q_norm": gain(ks[7], (L, hd)),
        "nsa_k_cmp_norm": gain(ks[8], (L, hd)),
        "nsa_k_slc_norm": gain(ks[9], (L, hd)),
        "nsa_k_win_norm": gain(ks[10], (L, hd)),
        "cmp_k_pos": 0.1 * jax.random.normal(ks[11], (L, CMP_BLOCK, hd), f32),
        "cmp_k_w1": nrm(ks[12], (L, CMP_BLOCK * hd, CMP_HIDDEN), CMP_BLOCK * hd),
        "cmp_k_w2": nrm(ks[13], (L, CMP_HIDDEN, hd), CMP_HIDDEN),
        "cmp_v_pos": 0.1 * jax.random.normal(ks[14], (L, CMP_BLOCK, hd), f32),
        "cmp_v_w1": nrm(ks[15], (L, CMP_BLOCK * hd, CMP_HIDDEN), CMP_BLOCK * hd),
        "cmp_v_w2": nrm(ks[16], (L, CMP_HIDDEN, hd), CMP_HIDDEN),
        "nsa_out_norm": gain(ks[17], (L, N_NSA_HEADS, hd)),
        "sb_out_norm": gain(ks[18], (L, N_SB_HEADS, hd)),
        "w_out": nrm(ks[19], (L, mix, D_MODEL), mix),
        "ffn2_norm": gain(ks[20], (L, D_MODEL)),
        "ffn2_w_gate": nrm(ks[21], (L, D_MODEL, D_FF), D_MODEL),
        "ffn2_w_up": nrm(ks[22], (L, D_MODEL, D_FF), D_MODEL),
        "ffn2_w_down": nrm(ks[23], (L, D_FF, D_MODEL), D_FF),
    }


def reference(x, positions, ffn1_norm, ffn1_w_gate, ffn1_w_up, ffn1_w_down, mix_norm, w_in,
              nsa_q_norm, nsa_k_cmp_norm, nsa_k_slc_norm, nsa_k_win_norm,
              cmp_k_pos, cmp_k_w1, cmp_k_w2, cmp_v_pos, cmp_v_w1, cmp_v_w2,
              nsa_out_norm, sb_out_norm, w_out, ffn2_norm, ffn2_w_gate, ffn2_w_up, ffn2_w_down):
    b, s, _ = x.shape
    hd = HEAD_DIM
    kv = N_NSA_KV * hd
    sizes = [N_NSA_HEADS * hd, kv, kv, kv, kv, kv, kv, N_NSA_HEADS * 3,
             N_SB_HEADS * hd, N_SB_HEADS * hd, N_SB_HEADS * hd]
    split_at = np.cumsum(sizes)[:-1].tolist()

    inv_freq = ROPE_THETA ** (-jnp.arange(0, hd, 2, dtype=jnp.float32) / hd)
    ang = positions.astype(jnp.float32)[..., None] * inv_freq
    cos = jnp.cos(ang)[:, :, None, :].astype(x.dtype)
    sin = jnp.sin(ang)[:, :, None, :].astype(x.dtype)

    for l in range(DEPTH):
        x = x + 0.5 * swiglu(rms_norm(x, ffn1_norm[l]), ffn1_w_gate[l], ffn1_w_up[l], ffn1_w_down[l])

        h = rms_norm(x, mix_norm[l])
        parts = jnp.split(h @ w_in[l], split_at, axis=-1)
        q_n, kc_t, vc_t, ks_t, vs_t, kw_t, vw_t, gate_n, q_s, k_s, v_s = parts
        kv_shape = (b, s, N_NSA_KV, hd)

        q_n = apply_rope(rms_norm(q_n.reshape(b, s, N_NSA_HEADS, hd), nsa_q_norm[l]), cos, sin)
        kc = compress_blocks(apply_rope(kc_t.reshape(kv_shape), cos, sin), cmp_k_pos[l], cmp_k_w1[l], cmp_k_w2[l])
        kc = rms_norm(kc, nsa_k_cmp_norm[l])
        vc = compress_blocks(vc_t.reshape(kv_shape), cmp_v_pos[l], cmp_v_w1[l], cmp_v_w2[l])
        k_slc = apply_rope(rms_norm(ks_t.reshape(kv_shape), nsa_k_slc_norm[l]), cos, sin)
        k_win = apply_rope(rms_norm(kw_t.reshape(kv_shape), nsa_k_win_norm[l]), cos, sin)
        o_nsa = nsa_attention(q_n, kc, vc, k_slc, vs_t.reshape(kv_shape), k_win,
                              vw_t.reshape(kv_shape), gate_n.reshape(b, s, N_NSA_HEADS, 3))

        sb_shape = (b, s, N_SB_HEADS, hd)
        o_sb = stick_breaking_attention(q_s.reshape(sb_shape), k_s.reshape(sb_shape), v_s.reshape(sb_shape))

        y = jnp.concatenate([rms_norm(o_nsa, nsa_out_norm[l]).reshape(b, s, -1),
                             rms_norm(o_sb, sb_out_norm[l]).reshape(b, s, -1)], axis=-1)
        x = x + y @ w_out[l]

        x = x + 0.5 * swiglu(rms_norm(x, ffn2_norm[l]), ffn2_w_gate[l], ffn2_w_up[l], ffn2_w_down[l])
    return x
```

```python
import numpy as np
import concourse.bass as bass
import concourse.mybir as mybir
from concourse.bass_utils import run_bass_kernel_spmd

F32 = mybir.dt.float32
BF16 = mybir.dt.bfloat16
I32 = mybir.dt.int32
AF = mybir.ActivationFunctionType
ALU = mybir.AluOpType
AX = mybir.AxisListType

D = 2048
DC = 16
FF = 5632
FCN = 44
S = 4096
NT = 32
HD = 128
NEG = -30000.0
SCALE = 128 ** -0.5
EPS = 1e-6
DEBUG = False
SAME_ENGINE_SYNC = True
KD = 8


PHASE_MARKS = []


class Res:
    __slots__ = ("w", "r")

    def __init__(self):
        self.w = None
        self.r = []


class Op:
    __slots__ = ("eng", "fn", "deps", "dma", "inc", "tok", "pre")

    def __init__(self, eng, fn, dma):
        self.eng = eng
        self.fn = fn
        self.dma = dma
        self.deps = []
        self.inc = False
        self.tok = None
        self.pre = None


class T:
    __slots__ = ("ap", "res")

    def __init__(self, ap, res=None):
        self.ap = ap
        self.res = res if res is not None else Res()


ENGS = ["pe", "act", "dve", "pool", "sp"]


class Sched:
    def __init__(self):
        self.ops = {e: [] for e in ENGS}
        self.dmas = {"sp": [], "pool": []}
        self.pending = {e: [] for e in ENGS}

    def add(self, eng, fn, reads=(), writes=(), dma=False):
        op = Op(eng, fn, dma)
        deps = {}
        for r in reads:
            if r.w is not None:
                deps[id(r.w)] = r.w
        for w in writes:
            if w.w is not None:
                deps[id(w.w)] = w.w
            for q in w.r:
                deps[id(q)] = q
        for p in self.pending[eng]:
            deps[id(p)] = p
        self.pending[eng] = []
        for r in reads:
            r.r.append(op)
        for w in writes:
            w.w = op
            w.r = []
        for d in deps.values():
            if d is op:
                continue
            if (not d.dma) and (not dma) and d.eng == eng:
                if eng == "pe" or not SAME_ENGINE_SYNC:
                    continue
            op.deps.append(d)
            if not d.dma:
                d.inc = True
        self.ops[eng].append(op)
        if dma:
            self.dmas[eng].append(op)
        return op

    def barrier(self):
        lst = []
        for e in ENGS:
            for op in reversed(self.ops[e]):
                if not op.dma:
                    lst.append(op)
                    break
        for q in ("sp", "pool"):
            lst.extend(self.dmas[q][-KD:])
        for e in ENGS:
            self.pending[e] = list(lst)

    def emit(self, nc, sems, dsems):
        for e in ENGS:
            k = 0
            for op in self.ops[e]:
                if (not op.dma) and op.inc:
                    k += 1
                    op.tok = (sems[e], k)
        alld = []
        for q in ("sp", "pool"):
            for i, op in enumerate(self.dmas[q]):
                op.tok = (dsems[q][i % KD], 16 * (i // KD + 1))
                if i >= KD:
                    op.pre = (dsems[q][i % KD], 16 * (i // KD))
            alld.extend(self.dmas[q][-KD:])
        ops = self.ops

        def run(e, eng):
            seen = {}
            for op in ops[e]:
                waits = [d.tok for d in op.deps]
                if op.pre is not None:
                    waits.append(op.pre)
                for sem, val in waits:
                    if seen.get(sem.num, 0) < val:
                        eng.wait_ge(sem, val)
                        seen[sem.num] = val
                ins = op.fn(eng)
                if op.dma:
                    ins.then_inc(op.tok[0], 16)
                elif op.inc:
                    ins.then_inc(op.tok[0], 1)
            if e == "sp":
                for d in alld:
                    sem, val = d.tok
                    if seen.get(sem.num, 0) < val:
                        eng.wait_ge(sem, val)
                        seen[sem.num] = val

        with nc.Block() as block:
            @block.tensor
            def _(t):
                run("pe", t)

            @block.scalar
            def _(s):
                run("act", s)

            @block.vector
            def _(v):
                run("dve", v)

            @block.gpsimd
            def _(g):
                run("pool", g)

            @block.sync
            def _(sy):
                run("sp", sy)


class Arena:
    def __init__(self, ap, ncols):
        self.ap = ap
        self.n = ncols
        self.off = 0

    def alloc(self, cols, dt=BF16):
        sz = 2 if dt == BF16 else 4
        n2 = (cols * sz + 63) // 64 * 32
        assert self.off + n2 <= self.n, ("arena overflow", self.off, n2, self.n)
        a = self.ap[:, self.off:self.off + n2]
        self.off += n2
        if dt == BF16:
            return a[:, 0:cols]
        return a.bitcast(dt)[:, 0:cols]

    def tile(self, cols, dt=BF16, res=None):
        return T(self.alloc(cols, dt), res)


def r3(ap, a):
    return ap.rearrange("p (a b) -> p a b", a=a)


def build_program():
    nc = bass.Bass("TRN2", target_bir_lowering=False)
    S_ = Sched()
    dkind = "ExternalOutput" if DEBUG else "Internal"

    def din(name, shape, dt=F32):
        return nc.dram_tensor(name, list(shape), dt, kind="ExternalInput").ap()

    def dscr(name, shape, dt):
        return nc.dram_tensor(name, list(shape), dt, kind=dkind).ap()

    x_b = din("x_b", [S, D])
    pos_in = din("pos", [128, NT], I32)
    sel_in = din("sel", [128, 2])
    ffw = {}
    for i in (1, 2):
        ffw[i] = (din(f"f{i}_wg", [D, FF]), din(f"f{i}_wu", [D, FF]), din(f"f{i}_wd", [FF, D]), din(f"f{i}_norm", [1, D]))
    mix_norm = din("mix_norm", [1, D])
    w_in_g = din("w_in_g", [2, D, 2828])
    g7_in = din("g7", [1, 896])
    kcn_in = din("kcn", [1, 128])
    nsaon_in = din("nsaon", [2, 512])
    sbon_in = din("sbon", [128, 8])
    cw1 = (din("ck_w1", [4096, 256]), din("cv_w1", [4096, 256]))
    cw2 = (din("ck_w2", [256, 128]), din("cv_w2", [256, 128]))
    cpos = (din("ck_pos", [32, 128]), din("cv_pos", [32, 128]))
    w_out = din("w_out", [D, D])
    ident_in = din("ident", [128, 128])
    invf_in = din("invf", [1, 64])
    tri4_in = din("tri4", [128, 512])
    tri24_in = din("tri24", [128, 512])
    sbm_in = din("sbm", [128, 2048])
    cbt_in = din("cbt", [256, 4096])
    mmat_in = din("mmat", [256, 65])
    atab_in = din("atab", [S, 64])
    btab_in = din("btab", [S, 64])
    ee_in = din("ee", [64, 4096])
    ust_in = din("ust", [128, 128])
    ule_in = din("ule", [128, 128])
    out_d = nc.dram_tensor("out", [2048, D], F32, kind="ExternalOutput").ap()

    x1_d = dscr("x1", [S, D], F32)
    hT_d = dscr("hT", [D, S], BF16)
    ropeT_d = dscr("ropeT", [2, 7, 128, S], BF16)
    featT_d = dscr("featT", [2, 9, 128, S], BF16)
    vsw_d = dscr("vsw", [2, S, 256], BF16)
    gat_d = dscr("gat", [2, S, 12], F32)
    vsb_d = dscr("vsb", [2, S, 512], BF16)
    yT_d = dscr("yT", [16, 128, S], BF16)
    x2_d = dscr("x2", [2048, D], F32)
    cs_d = dscr("cs", [2, 128, NT * 64], F32)
    dbg_kc = dscr("dbg_kc", [2, 128, 256], BF16) if DEBUG else None
    dbg_vc = dscr("dbg_vc", [2, 256, 193], BF16) if DEBUG else None

    dres_map = {}

    def dres(*key):
        r = dres_map.get(key)
        if r is None:
            r = Res()
            dres_map[key] = r
        return r

    ARENA_COLS = 90 * 1024
    import contextlib
    with contextlib.ExitStack() as es:
        arena_t = es.enter_context(nc.sbuf_tensor("arena", [128, ARENA_COLS], BF16))
        banks = []
        for i in range(8):
            pt = es.enter_context(nc.psum_tensor(f"bank{i}", [128, 512], F32))
            banks.append(T(pt[:]))
        sems = {}
        for e in ("pe", "act", "dve", "pool", "sp"):
            sems[e] = es.enter_context(nc.semaphore("s_" + e))
        dsems = {q: [es.enter_context(nc.semaphore(f"d_{q}{i}")) for i in range(KD)] for q in ("sp", "pool")}

        AR = Arena(arena_t[:], ARENA_COLS)

        def rl(ts):
            return [t.res if isinstance(t, T) else t for t in ts]

        def dma(q, out, in_, reads, writes):
            return S_.add(q, lambda e: e.dma_start(out=out, in_=in_), rl(reads), rl(writes), dma=True)

        def mm(out, lhsT, rhs, start, stop, reads, writes):
            return S_.add("pe", lambda e: e.matmul(out, lhsT, rhs, start=start, stop=stop, skip_group_check=True),
                          rl(reads), rl(writes))

        def tr(out, in_, idn, reads, writes):
            return S_.add("pe", lambda e: e.transpose(out, in_, idn), rl(reads), rl(writes))

        def act(out, in_, func, reads, writes, bias=None, scale=1.0, accum=None):
            def f(e):
                kw = {}
                if bias is not None:
                    np_ = out.shape[0]
                    kw["bias"] = bias if bias.shape[0] == np_ else bias[0:np_, :]
                if accum is not None:
                    kw["accum_out"] = accum
                return e.activation(out=out, in_=in_, func=func, scale=scale, **kw)
            return S_.add("act", f, rl(reads), rl(writes))

        def tt_(eng, out, in0, in1, op, reads, writes):
            return S_.add(eng, lambda e: e.tensor_tensor(out=out, in0=in0, in1=in1, op=op), rl(reads), rl(writes))

        def ts_(eng, out, in0, s1, s2, op0, op1, reads, writes):
            if op1 is None:
                return S_.add(eng, lambda e: e.tensor_scalar(out=out, in0=in0, scalar1=s1, scalar2=None, op0=op0),
                              rl(reads), rl(writes))
            return S_.add(eng, lambda e: e.tensor_scalar(out=out, in0=in0, scalar1=s1, scalar2=s2, op0=op0, op1=op1),
                          rl(reads), rl(writes))

        def stt(eng, out, in0, scalar, in1, op0, op1, reads, writes):
            return S_.add(eng, lambda e: e.scalar_tensor_tensor(out=out, in0=in0, scalar=scalar, in1=in1, op0=op0, op1=op1),
                          rl(reads), rl(writes))

        def cp(eng, out, in_, reads, writes):
            if eng == "act":
                return S_.add("act", lambda e: e.copy(out=out, in_=in_), rl(reads), rl(writes))
            return S_.add(eng, lambda e: e.tensor_copy(out=out, in_=in_), rl(reads), rl(writes))

        def mset(eng, ap, val, writes):
            return S_.add(eng, lambda e: e.memset(ap, val), [], rl(writes))

        def rsum(out, in_, reads, writes):
            return S_.add("dve", lambda e: e.reduce_sum(out=out, in_=in_, axis=AX.X), rl(reads), rl(writes))

        def recip(out, in_, reads, writes):
            return S_.add("dve", lambda e: e.reciprocal(out=out, in_=in_), rl(reads), rl(writes))

        def bankbf(b):
            return banks[b].ap.bitcast(BF16)

        ident = AR.tile(128)
        dma("pool", ident.ap, ident_in[:, :], [], [ident])
        eps_t = AR.tile(1, F32)
        one_t = AR.tile(1, F32)
        n2pi_t = AR.tile(1, F32)
        mset("dve", eps_t.ap, EPS, [eps_t])
        mset("dve", one_t.ap, 1.0, [one_t])
        mset("dve", n2pi_t.ap, -6.283185, [n2pi_t])
        sel_t = AR.tile(2, F32)
        dma("sp", sel_t.ap, sel_in[:, :], [], [sel_t])
        PERSIST = AR.off

        def phase_reset():
            S_.barrier()
            AR.off = PERSIST
            PHASE_MARKS.append({e: len(S_.ops[e]) for e in ENGS})

        def rstd_lnexp(out_ap, ss_ap, inv_n, reads, writes, tmp):
            act(tmp.ap, ss_ap, AF.Ln, reads, [tmp], bias=eps_t.ap, scale=inv_n)
            act(out_ap, tmp.ap, AF.Exp, [tmp], writes, scale=-0.5)

        def norm_transpose(src_ap, src_res, xt, gt, junk, xn, ss, rt, dst_ap_fn, dst_res, blend=None):
            if blend is None:
                dma("sp", xt.ap, src_ap, src_res, [xt])
            else:
                blend()
            mset("dve", ss.ap, 0.0, [ss])
            act(junk.ap, xt.ap, AF.Square, [xt, ss], [junk, ss], accum=ss.ap)
            act(rt.ap, ss.ap, AF.Sqrt, [ss], [rt], bias=eps_t.ap, scale=1.0 / D)
            recip(rt.ap, rt.ap, [rt], [rt])
            stt("dve", xn.ap, xt.ap, rt.ap, gt.ap, ALU.mult, ALU.mult, [xt, rt, gt], [xn])
            for hb in range(2):
                b = 6 + hb
                pb = bankbf(b)
                for j in range(8):
                    dc = hb * 8 + j
                    tr(pb[:, j * 128:(j + 1) * 128], xn.ap[:, dc * 128:(dc + 1) * 128], ident.ap, [xn, ident], [banks[b]])
                cp("act" if hb == 0 else "dve", dst_ap_fn(hb * 8, 8), r3(pb[:, 0:1024], 8), [banks[b]], dst_res)

        def ffn_phase(tag, x_src, src_res_fn, ntok, wts, dst_ap, dst_res_fn, then_norm=None):
            wg_d, wu_d, wd_d, nrm_d = wts
            phase_reset()
            xnT = AR.alloc(16 * 1024)
            xnT3 = r3(xnT, 16)
            xn_res = [[Res() for _ in range(2)] for _ in range(8)]
            actT = AR.alloc(44 * 1024)
            actT3 = r3(actT, 44)
            act_res = [[Res() for _ in range(2)] for _ in range(44)]
            slots = [AR.tile(4096) for _ in range(4)]
            wdb = [AR.tile(2048) for _ in range(2)]
            silu_t = [AR.tile(512, F32) for _ in range(2)]
            NXR = 6
            xres_t = [AR.tile(512, F32) for _ in range(NXR)]
            xrc = [0]

            def load_res(tg, dmc, t8):
                row0 = tg * 1024 + t8 * 128
                xr_ = xres_t[(xrc[0] + t8) % NXR]
                dma("sp", xr_.ap, x_src[row0:row0 + 128, dmc * 512:(dmc + 1) * 512], src_res_fn(row0 // 128), [xr_])
            ss = AR.tile(1, F32)
            rt = AR.tile(1, F32)
            xtA = T(slots[0].ap.bitcast(F32), slots[0].res)
            xtB = T(slots[1].ap.bitcast(F32), slots[1].res)
            gt = T(slots[2].ap.bitcast(F32), slots[2].res)
            xn = T(slots[3].ap[:, 0:2048], slots[3].res)
            junk = T(slots[3].ap[:, 2048:4096], slots[3].res)
            wg_v = wg_d.rearrange("(c p) f -> p c f", p=128)
            wu_v = wu_d.rearrange("(c p) f -> p c f", p=128)
            ngrp = ntok // 1024
            for tg in range(ngrp):
                dma("sp", gt.ap, nrm_d[0:1, :].partition_broadcast(128), [], [gt])
                for t8 in range(8):
                    row0 = tg * 1024 + t8 * 128
                    xt = xtA if t8 % 2 == 0 else xtB

                    def dst_fn(dc0, n, t8=t8):
                        return xnT3[:, dc0:dc0 + n, t8 * 128:(t8 + 1) * 128]
                    norm_transpose(x_src[row0:row0 + 128, :], src_res_fn(row0 // 128), xt, gt, junk, xn, ss, rt,
                                   dst_fn, [xn_res[t8][0], xn_res[t8][1]])
                for fp in range(22):
                    wgt = slots[(fp % 2) * 2]
                    wut = slots[(fp % 2) * 2 + 1]
                    dma("pool", r3(wgt.ap, 16), wg_v[:, :, fp * 256:(fp + 1) * 256], [], [wgt])
                    dma("pool", r3(wut.ap, 16), wu_v[:, :, fp * 256:(fp + 1) * 256], [], [wut])
                    for j in range(2):
                        fc = fp * 2 + j
                        bs = 4 * (fc % 2)
                        for (wt, boff) in ((wgt, 0), (wut, 2)):
                            w3 = r3(wt.ap, 16)
                            for dc in range(16):
                                for hf in range(2):
                                    mm(banks[bs + boff + hf].ap, w3[:, dc, j * 128:(j + 1) * 128],
                                       xnT3[:, dc, hf * 512:(hf + 1) * 512], dc == 0, dc == 15,
                                       [wt] + [xn_res[hf * 4 + q][dc // 8] for q in range(4)], [banks[bs + boff + hf]])
                        for hf in range(2):
                            st_ = silu_t[hf]
                            act(st_.ap, banks[bs + hf].ap, AF.Silu, [banks[bs + hf]], [st_])
                            tt_("dve", actT3[:, fc, hf * 512:(hf + 1) * 512], st_.ap, banks[bs + 2 + hf].ap, ALU.mult,
                                [st_, banks[bs + 2 + hf]], [act_res[fc][hf]])
                for dmc in range(4):
                    for t8 in range(NXR):
                        load_res(tg, dmc, t8)
                    for fq in range(11):
                        wdt = wdb[fq % 2]
                        dma("pool", r3(wdt.ap, 4),
                            wd_d[fq * 512:(fq + 1) * 512, dmc * 512:(dmc + 1) * 512].rearrange("(j p) c -> p j c", p=128),
                            [], [wdt])
                        w3 = r3(wdt.ap, 4)
                        for j in range(4):
                            fc = fq * 4 + j
                            for t8 in range(8):
                                mm(banks[t8].ap, actT3[:, fc, t8 * 128:(t8 + 1) * 128], w3[:, j, :], fc == 0, fc == 43,
                                   [wdt, act_res[fc][t8 // 4]], [banks[t8]])
                    for t8 in range(8):
                        row0 = tg * 1024 + t8 * 128
                        xr_ = xres_t[(xrc[0] + t8) % NXR]
                        stt("dve", xr_.ap, banks[t8].ap, 0.5, xr_.ap, ALU.mult, ALU.add, [banks[t8], xr_], [xr_])
                        dma("sp", dst_ap[row0:row0 + 128, dmc * 512:(dmc + 1) * 512], xr_.ap, [xr_], [dst_res_fn(row0 // 128, dmc)])
                        if t8 + NXR < 8:
                            load_res(tg, dmc, t8 + NXR)
                    xrc[0] += 8

        ffn_phase("f1", x_b, lambda tt: [], S, ffw[1], x1_d, lambda tt, dmc: dres("x1", tt, dmc))

        phase_reset()
        gt = AR.tile(2048, F32)
        dma("sp", gt.ap, mix_norm[0:1, :].partition_broadcast(128), [], [gt])
        xts = [AR.tile(2048, F32) for _ in range(2)]
        xn = AR.tile(2048)
        junk = AR.tile(2048)
        ss = AR.tile(1, F32)
        rt = AR.tile(1, F32)
        hb_t = [AR.tile(16 * 512) for _ in range(2)]
        hT_v = hT_d.rearrange("(c p) t -> p c t", p=128)
        for tg in range(8):
            hb = hb_t[tg % 2]
            hb3 = r3(hb.ap, 16)
            for t4 in range(4):
                tt = tg * 4 + t4

                def dst_fn(dc0, n, t4=t4, hb3=hb3):
                    return hb3[:, dc0:dc0 + n, t4 * 128:(t4 + 1) * 128]
                norm_transpose(x1_d[tt * 128:(tt + 1) * 128, :], [dres("x1", tt, q) for q in range(4)], xts[tt % 2], gt,
                               junk, xn, ss, rt, dst_fn, [hb])
            dma("sp", hT_v[:, :, tg * 512:(tg + 1) * 512], hb3, [hb], [dres("hT", tg)])

        phase_reset()
        invf = AR.tile(64, F32)
        dma("sp", invf.ap, invf_in[0:1, :].partition_broadcast(128), [], [invf])
        posi = AR.tile(NT, I32)
        dma("sp", posi.ap, pos_in[:, :], [], [posi])
        posf = AR.tile(NT, F32)
        cp("dve", posf.ap, posi.ap, [posi], [posf])
        ang0 = AR.tile(NT * 64, F32)
        ta = AR.tile(NT * 64, F32)
        tb = AR.tile(NT * 64, F32)
        ki = AR.tile(NT * 64, I32)
        tab = AR.tile(NT * 64, F32)
        tt_("dve", r3(ang0.ap, NT), posf.ap.unsqueeze(2).broadcast_to([128, NT, 64]),
            invf.ap.unsqueeze(1).broadcast_to([128, NT, 64]), ALU.mult, [posf, invf], [ang0])
        for ci, shift in ((0, 0.0), (1, 1.5707963267948966)):
            ts_("dve", ta.ap, ang0.ap, shift, None, ALU.add, None, [ang0, ta], [ta])
            ts_("dve", tb.ap, ta.ap, 0.15915494309189535, None, ALU.mult, None, [ta, tb], [tb])
            cp("dve", ki.ap, tb.ap, [tb, ki], [ki])
            cp("dve", tb.ap, ki.ap, [ki], [tb])
            stt("dve", ta.ap, tb.ap, -6.28125, ta.ap, ALU.mult, ALU.add, [tb, ta], [ta])
            stt("dve", ta.ap, tb.ap, -0.0019353071795864769, ta.ap, ALU.mult, ALU.add, [tb, ta], [ta])
            ts_("dve", tb.ap, ta.ap, 3.14159265, None, ALU.is_gt, None, [ta], [tb])
            stt("dve", ta.ap, tb.ap, -6.283185307179586, ta.ap, ALU.mult, ALU.add, [tb, ta], [ta])
            ts_("dve", ta.ap, ta.ap, 3.1415925, 6.283185, ALU.min, ALU.add, [ta], [ta])
            act(tab.ap, ta.ap, AF.Sin, [ta, n2pi_t, tab], [tab], bias=n2pi_t.ap, scale=1.0)
            dma("sp", cs_d[ci, :, :], tab.ap, [tab], [dres("cs", ci)])

        for g in range(2):
            phase_reset()
            W = AR.tile(16 * 2828)
            W3 = r3(W.ap, 16)
            wv = w_in_g[g].rearrange("(c p) f -> p c f", p=128)
            dma("pool", W3[:, 0:8, :], wv[:, 0:8, :], [], [W])
            dma("pool", W3[:, 8:16, :], wv[:, 8:16, :], [W], [W])
            g7 = AR.tile(896, F32)
            dma("sp", g7.ap, g7_in[0:1, :].partition_broadcast(128), [], [g7])
            cosT = AR.tile(NT * 64, F32)
            sinT = AR.tile(NT * 64, F32)
            cos3 = r3(cosT.ap, NT)
            sin3 = r3(sinT.ap, NT)
            dma("sp", sinT.ap, cs_d[0, :, :], [dres("cs", 0)], [sinT])
            dma("sp", cosT.ap, cs_d[1, :, :], [dres("cs", 1)], [cosT])
            hbuf = [AR.tile(16 * 512) for _ in range(2)]
            fstage = [AR.tile(512) for _ in range(2)]
            xr = AR.tile(896, F32)
            sq = AR.tile(896, F32)
            ss7 = AR.tile(7, F32)
            l7 = AR.tile(7, F32)
            t1 = AR.tile(448, F32)
            t2 = AR.tile(448, F32)
            t3 = AR.tile(448, F32)
            t4t = AR.tile(448, F32)
            ro = AR.tile(896)
            rstage = [AR.tile(7 * 512) for _ in range(1)]
            vstage = [AR.tile(256) for _ in range(2)]
            gexp = AR.tile(12, F32)
            gstage = [AR.tile(12, F32) for _ in range(2)]
            cstage = [AR.tile(512) for _ in range(2)]
            ropeT_v = ropeT_d[g].rearrange("i p t -> p i t")
            ro_t = [ro, AR.tile(896)]
            pend = None

            def emit_tr(pd, rs, rs3):
                ro_, t4_ = pd
                pb = bankbf(6)
                for i in range(7):
                    tr(pb[:, i * 128:(i + 1) * 128], ro_.ap[:, i * 128:(i + 1) * 128], ident.ap, [ro_, ident], [banks[6]])
                cp("act", rs3[:, :, t4_ * 128:(t4_ + 1) * 128], r3(pb[:, 0:896], 7), [banks[6]], [rs])
            dma("sp", r3(hbuf[0].ap, 16), hT_v[:, :, 0:512], [dres("hT", 0)], [hbuf[0]])
            for tg in range(8):
                hb = hbuf[tg % 2]
                hb3 = r3(hb.ap, 16)
                if tg + 1 < 8:
                    dma("sp", r3(hbuf[(tg + 1) % 2].ap, 16), hT_v[:, :, (tg + 1) * 512:(tg + 2) * 512], [dres("hT", tg + 1)],
                        [hbuf[(tg + 1) % 2]])
                for ch in range(9):
                    b = 4 + (ch % 2)
                    c0 = 1676 + ch * 128
                    for dc in range(16):
                        mm(banks[b].ap, W3[:, dc, c0:c0 + 128], hb3[:, dc, :], dc == 0, dc == 15, [W, hb], [banks[b]])
                    fs = fstage[ch % 2]
                    sc_ = SCALE if 1 <= ch <= 4 else 1.0
                    S_.add("act", lambda e, o=fs.ap, i=banks[b].ap, s=sc_: e.mul(out=o, in_=i, mul=s), rl([banks[b]]), rl([fs]))
                    dma("sp", featT_d[g, ch, :, tg * 512:(tg + 1) * 512], fs.ap, [fs], [dres("featT", g, ch, tg)])
                rs = rstage[0]
                rs3 = r3(rs.ap, 7)
                for t4 in range(4):
                    tt = tg * 4 + t4
                    for dc in range(16):
                        l_ = hb3[:, dc, t4 * 128:(t4 + 1) * 128]
                        mm(banks[0].ap, l_, W3[:, dc, 0:512], dc == 0, dc == 15, [W, hb], [banks[0]])
                        mm(banks[1].ap[:, 0:384], l_, W3[:, dc, 512:896], dc == 0, dc == 15, [W, hb], [banks[1]])
                        mm(banks[2].ap[:, 0:268], l_, W3[:, dc, 896:1164], dc == 0, dc == 15, [W, hb], [banks[2]])
                        mm(banks[3].ap, l_, W3[:, dc, 1164:1676], dc == 0, dc == 15, [W, hb], [banks[3]])
                    if pend is not None:
                        emit_tr(pend, rs, rs3)
                        pend = None
                    ro = ro_t[tt % 2]
                    vs_ = vstage[tt % 2]
                    cp("dve", vs_.ap, banks[2].ap[:, 0:256], [banks[2]], [vs_])
                    dma("sp", vsw_d[g, tt * 128:(tt + 1) * 128, :], vs_.ap, [vs_], [dres("vsw", g, tt)])
                    gs = gstage[tt % 2]
                    act(gexp.ap, banks[2].ap[:, 256:268], AF.Exp, [banks[2]], [gexp], scale=-1.0)
                    ts_("dve", gexp.ap, gexp.ap, 1.0, None, ALU.add, None, [gexp], [gexp])
                    recip(gs.ap, gexp.ap, [gexp], [gs])
                    dma("sp", gat_d[g, tt * 128:(tt + 1) * 128, :], gs.ap, [gs], [dres("gat", g, tt)])
                    cs = cstage[tt % 2]
                    cp("act", cs.ap, banks[3].ap, [banks[3]], [cs])
                    dma("sp", vsb_d[g, tt * 128:(tt + 1) * 128, :], cs.ap, [cs], [dres("vsb", g, tt)])
                    cp("act", xr.ap[:, 0:512], banks[0].ap, [banks[0]], [xr])
                    cp("dve", xr.ap[:, 512:896], banks[1].ap[:, 0:384], [banks[1], xr], [xr])
                    tt_("dve", sq.ap, xr.ap, xr.ap, ALU.mult, [xr], [sq])
                    rsum(ss7.ap, r3(sq.ap, 7), [sq], [ss7])
                    rstd_lnexp(ss7.ap, ss7.ap, 1.0 / 128, [ss7], [ss7], l7)
                    mset("dve", ss7.ap[:, 4:5], 1.0, [ss7])
                    tt_("dve", r3(sq.ap, 7), r3(xr.ap, 7), ss7.ap.unsqueeze(2).broadcast_to([128, 7, 128]), ALU.mult,
                        [xr, ss7], [sq])
                    tt_("dve", xr.ap, sq.ap, g7.ap, ALU.mult, [sq, g7], [xr])
                    x3 = r3(xr.ap, 7)
                    x1v = x3[:, :, 0:64]
                    x2v = x3[:, :, 64:128]
                    cb = cos3[:, tt, :].unsqueeze(1).broadcast_to([128, 7, 64])
                    sb_ = sin3[:, tt, :].unsqueeze(1).broadcast_to([128, 7, 64])
                    ro3 = r3(ro.ap, 7)
                    tt_("dve", r3(t1.ap, 7), x1v, cb, ALU.mult, [xr, cosT], [t1])
                    tt_("dve", r3(t2.ap, 7), x2v, sb_, ALU.mult, [xr, sinT], [t2])
                    tt_("dve", ro3[:, :, 0:64], r3(t1.ap, 7), r3(t2.ap, 7), ALU.subtract, [t1, t2], [ro])
                    tt_("pool", r3(t3.ap, 7), x2v, cb, ALU.mult, [xr, cosT], [t3])
                    tt_("pool", r3(t4t.ap, 7), x1v, sb_, ALU.mult, [xr, sinT], [t4t])
                    tt_("pool", ro3[:, :, 64:128], r3(t3.ap, 7), r3(t4t.ap, 7), ALU.add, [t3, t4t, ro], [ro])
                    pend = (ro, t4)
                emit_tr(pend, rs, rs3)
                pend = None
                dma("sp", ropeT_v[:, :, tg * 512:(tg + 1) * 512], rs3, [rs], [dres("ropeT", g, tg)])

            phase_reset()
            rope_all = [dres("ropeT", g, tg) for tg in range(8)]
            kcT_s = AR.tile(256)
            vc_aug = [AR.tile(193) for _ in range(2)]
            cmp_mark = AR.off
            kT = AR.tile(S)
            vT = AR.tile(S)
            dma("sp", kT.ap, ropeT_d[g, 4, :, :], rope_all, [kT])
            dma("sp", vT.ap, featT_d[g, 0, :, :], [dres("featT", g, 0, tg) for tg in range(8)], [vT])
            kcn_t = AR.tile(128, F32)
            dma("sp", kcn_t.ap, kcn_in[0:1, :].partition_broadcast(128), [], [kcn_t])
            for nt in range(2):
                dma("pool", vc_aug[nt].ap[:, 128:193], mmat_in[nt * 128:(nt + 1) * 128, :], [], [vc_aug[nt]])
            for kv in range(2):
                src = kT if kv == 0 else vT
                w1 = AR.tile(32 * 256)
                w13 = r3(w1.ap, 32)
                dma("pool", w13, cw1[kv].rearrange("(l d) h -> d l h", d=128), [], [w1])
                w2 = AR.tile(2 * 128)
                w23 = r3(w2.ap, 2)
                dma("pool", w23, cw2[kv].rearrange("(c p) d -> p c d", p=128), [], [w2])
                pst = AR.tile(128)
                dma("pool", pst.ap[0:32, :], cpos[kv][:, :], [], [pst])
                posT = AR.tile(32)
                pb = bankbf(7)
                tr(pb[:, 0:32], pst.ap[0:32, :], ident.ap[0:32, 0:32], [pst, ident], [banks[7]])
                cp("dve", posT.ap, pb[:, 0:32], [banks[7]], [posT])
                cst = AR.tile(2, F32)
                gT = [AR.tile(256) for _ in range(2)]
                u = AR.tile(256, F32)
                u2 = AR.tile(256, F32)
                th = AR.tile(256, F32)
                for hc in range(2):
                    for l in range(32):
                        mm(banks[5].ap[:, hc:hc + 1], w13[:, l, hc * 128:(hc + 1) * 128], posT.ap[:, l:l + 1], l == 0, l == 31,
                           [w1, posT], [banks[5]])
                cp("dve", cst.ap, banks[5].ap[:, 0:2], [banks[5]], [cst])
                for hc in range(2):
                    b = 4
                    for l in range(32):
                        mm(banks[b].ap[:, 0:255], w13[:, l, hc * 128:(hc + 1) * 128], src.ap[:, l:l + 16 * 254 + 1:16],
                           l == 0, l == 31, [w1, src], [banks[b]])
                    act(u.ap[:, 0:255], banks[b].ap[:, 0:255], AF.Identity, [banks[b], cst], [u], bias=cst.ap[:, hc:hc + 1])
                    tt_("dve", u2.ap[:, 0:255], u.ap[:, 0:255], u.ap[:, 0:255], ALU.mult, [u], [u2])
                    ts_("dve", u2.ap[:, 0:255], u2.ap[:, 0:255], 0.044715, 1.0, ALU.mult, ALU.add, [u2], [u2])
                    tt_("dve", u2.ap[:, 0:255], u2.ap[:, 0:255], u.ap[:, 0:255], ALU.mult, [u2, u], [u2])
                    act(th.ap[:, 0:255], u2.ap[:, 0:255], AF.Tanh, [u2], [th], scale=0.7978845608028654)
                    stt("dve", gT[hc].ap[:, 0:255], th.ap[:, 0:255], 1.0, u.ap[:, 0:255], ALU.add, ALU.mult, [th, u], [gT[hc]])
                for nt in range(2):
                    rows = 128 if nt == 0 else 127
                    b = 5
                    for hc in range(2):
                        mm(banks[b].ap[0:rows, 0:128], gT[hc].ap[:, nt * 128:nt * 128 + rows], w23[:, hc, :], hc == 0, hc == 1,
                           [gT[hc], w2], [banks[b]])
                    if kv == 0:
                        kc = AR.tile(128, F32)
                        kq = AR.tile(128, F32)
                        kss = AR.tile(1, F32)
                        kl = AR.tile(1, F32)
                        kb = AR.tile(128)
                        S_.add("act", lambda e, o=kc.ap[0:rows, :], i=banks[b].ap[0:rows, 0:128]: e.mul(out=o, in_=i, mul=0.5),
                               rl([banks[b]]), rl([kc]))
                        tt_("dve", kq.ap[0:rows, :], kc.ap[0:rows, :], kc.ap[0:rows, :], ALU.mult, [kc], [kq])
                        rsum(kss.ap[0:rows, :], kq.ap[0:rows, :], [kq], [kss])
                        rstd_lnexp(kss.ap[0:rows, :], kss.ap[0:rows, :], 1.0 / 128, [kss], [kss], T(kl.ap[0:rows, :], kl.res))
                        stt("dve", kb.ap[0:rows, :], kc.ap[0:rows, :], kss.ap[0:rows, :], kcn_t.ap[0:rows, :], ALU.mult, ALU.mult,
                            [kc, kss, kcn_t], [kb])
                        pb = bankbf(7)
                        tr(pb[:, 0:rows], kb.ap[0:rows, :], ident.ap[0:rows, 0:rows], [kb, ident], [banks[7]])
                        cp("dve", kcT_s.ap[:, nt * 128:nt * 128 + rows], pb[:, 0:rows], [banks[7]], [kcT_s])
                    else:
                        S_.add("act", lambda e, o=vc_aug[nt].ap[0:rows, 0:128], i=banks[b].ap[0:rows, 0:128]: e.mul(out=o, in_=i, mul=0.5),
                               rl([banks[b]]), rl([vc_aug[nt]]))
            if DEBUG:
                dma("sp", dbg_kc[g, :, :], kcT_s.ap, [kcT_s], [dres("dbgkc", g)])
                for nt in range(2):
                    dma("sp", dbg_vc[g, nt * 128:(nt + 1) * 128, :], vc_aug[nt].ap, [vc_aug[nt]], [dres("dbgvc", g, nt)])
            S_.barrier()
            AR.off = cmp_mark

            qT = AR.tile(4 * S)
            qT3 = r3(qT.ap, 4)
            dma("sp", qT3, ropeT_d[g, 0:4, :, :].rearrange("i p t -> p i t"), rope_all, [qT])
            ksT = AR.tile(S)
            kwT = AR.tile(S)
            dma("sp", ksT.ap, ropeT_d[g, 5, :, :], rope_all, [ksT])
            dma("sp", kwT.ap, ropeT_d[g, 6, :, :], rope_all, [kwT])
            vsa = AR.tile(NT * 129)
            vwa = AR.tile(NT * 129)
            vsa3 = r3(vsa.ap, NT)
            vwa3 = r3(vwa.ap, NT)
            vsw_all = [dres("vsw", g, tt) for tt in range(NT)]
            vsw_v = vsw_d[g].rearrange("(n p) c -> p n c", p=128)
            dma("sp", vsa3[:, :, 0:128], vsw_v[:, :, 0:128], vsw_all, [vsa])
            dma("sp", vwa3[:, :, 0:128], vsw_v[:, :, 128:256], vsw_all, [vwa])
            mset("dve", vsa3[:, :, 128:129], 1.0, [vsa])
            mset("dve", vwa3[:, :, 128:129], 1.0, [vwa])
            gat = AR.tile(NT * 12, F32)
            gat3 = r3(gat.ap, NT)
            dma("sp", gat3, gat_d[g].rearrange("(n p) c -> p n c", p=128), [dres("gat", g, tt) for tt in range(NT)], [gat])
            cbt = AR.tile(2 * S)
            cbt3 = r3(cbt.ap, 2)
            dma("pool", cbt3, cbt_in.rearrange("(n p) t -> p n t", p=128), [], [cbt])
            atab = AR.tile(NT * 64, F32)
            btab = AR.tile(NT * 64, F32)
            dma("sp", r3(atab.ap, NT), atab_in.rearrange("(n p) c -> p n c", p=128), [], [atab])
            dma("sp", r3(btab.ap, NT), btab_in.rearrange("(n p) c -> p n c", p=128), [], [btab])
            ee = AR.tile(S)
            dma("pool", ee.ap[0:64, :], ee_in[:, :], [], [ee])
            tri4 = AR.tile(512)
            tri24 = AR.tile(512)
            dma("pool", tri4.ap, tri4_in[:, :], [], [tri4])
            dma("pool", tri24.ap, tri24_in[:, :], [], [tri24])
            gon = AR.tile(512, F32)
            dma("sp", gon.ap, nsaon_in[g:g + 1, :].partition_broadcast(128), [], [gon])
            Pc = [AR.tile(512) for _ in range(2)]
            Ps = [AR.tile(512) for _ in range(4)]
            msk_t = [AR.tile(128) for _ in range(4)]
            mres = [Res() for _ in range(4)]
            SBK = [0, 1, 6]
            oc = AR.tile(4 * 193, F32)
            oc3 = r3(oc.ap, 4)
            osl = AR.tile(4 * 129, F32)
            osl3 = r3(osl.ap, 4)
            owi = AR.tile(4 * 129, F32)
            owi3 = r3(owi.ap, 4)
            rd = AR.tile(12, F32)
            rd3 = r3(rd.ap, 3)
            cf = AR.tile(12, F32)
            cf3 = r3(cf.ap, 3)
            psl = AR.tile(64, F32)
            sc = AR.tile(64, F32)
            sc2 = AR.tile(64, F32)
            mx = AR.tile(16, F32)
            selb = AR.tile(64)
            selT4 = AR.tile(512)
            o1 = AR.tile(512, F32)
            o2 = AR.tile(512, F32)
            o3 = AR.tile(512, F32)
            ssq = AR.tile(4, F32)
            l4 = AR.tile(4, F32)
            yb = AR.tile(512)
            ystage = [AR.tile(4 * 512) for _ in range(2)]
            yT_v = yT_d.rearrange("c p t -> p c t")
            PSI = [0]
            oc_t = [oc, AR.tile(4 * 193, F32)]
            rdc_t = [AR.tile(4, F32) for _ in range(2)]
            selT4_t = [selT4, AR.tile(512)]

            CHAIN = []
            X2 = [None]
            YTR = [None]
            yb_t = [yb, AR.tile(512)]

            def stage_x(tt):
                Q = qT3[:, :, tt * 128:(tt + 1) * 128]
                oc = oc_t[tt % 2]
                oc3 = r3(oc.ap, 4)
                rdc = rdc_t[tt % 2]
                selT4 = selT4_t[tt % 2]
                nts = [0] if tt < 16 else [0, 1]
                for nt in nts:
                    rows = 128 if nt == 0 else 127
                    needm = (nt == 1) or (tt <= 16)
                    mm(r3(banks[nt].ap[0:rows, :], 4), kcT_s.ap[:, nt * 128:nt * 128 + rows], Q, True, not needm,
                       [kcT_s, qT], [banks[nt]])
                    if needm:
                        for r in range(4):
                            mm(banks[nt].ap[0:rows, r * 128:(r + 1) * 128], ident.ap[0:rows, 0:rows],
                               cbt3[0:rows, nt, tt * 128:(tt + 1) * 128], False, r == 3, [ident, cbt], [banks[nt]])
                    act(Pc[nt].ap[0:rows, :], banks[nt].ap[0:rows, :], AF.Exp, [banks[nt]], [Pc[nt]], scale=SCALE)
                for r in range(4):
                    b = 2 + r
                    for i, nt in enumerate(nts):
                        rows = 128 if nt == 0 else 127
                        mm(banks[b].ap[:, 0:193], Pc[nt].ap[0:rows, r * 128:(r + 1) * 128],
                           vc_aug[nt].ap[0:rows, :], i == 0, i == len(nts) - 1, [Pc[nt], vc_aug[nt]], [banks[b]])
                for r in range(4):
                    cp("act" if r % 2 == 0 else "dve", oc3[:, r, :], banks[2 + r].ap[:, 0:193], [banks[2 + r], oc], [oc])
                ch = CHAIN
                ch.append(lambda: ts_("dve", rdc.ap, oc3[:, :, 192], 1e-30, None, ALU.max, None, [oc], [rdc]))
                ch.append(lambda: recip(rdc.ap, rdc.ap, [rdc], [rdc]))
                ch.append(lambda: ts_("dve", psl.ap, oc3[:, 0, 128:192], rdc.ap[:, 0:1], None, ALU.mult, None, [oc, rdc], [psl]))
                for r in range(1, 4):
                    ch.append(lambda r=r: stt("dve", psl.ap, oc3[:, r, 128:192], rdc.ap[:, r:r + 1], psl.ap, ALU.mult, ALU.add,
                                              [oc, rdc, psl], [psl]))
                ch.append(lambda: tt_("dve", sc.ap, psl.ap, r3(atab.ap, NT)[:, tt, :], ALU.mult, [psl, atab], [sc]))
                ch.append(lambda: tt_("dve", sc.ap, sc.ap, r3(btab.ap, NT)[:, tt, :], ALU.add, [sc, btab], [sc]))
                ch.append(lambda: S_.add("dve", lambda e: e.max(out=mx.ap[:, 0:8], in_=sc.ap), rl([sc]), rl([mx])))
                ch.append(lambda: S_.add("dve", lambda e: e.match_replace(out=sc2.ap, in_to_replace=mx.ap[:, 0:8], in_values=sc.ap,
                                                                          imm_value=-1e30), rl([sc, mx]), rl([sc2])))
                ch.append(lambda: S_.add("dve", lambda e: e.max(out=mx.ap[:, 8:16], in_=sc2.ap), rl([sc2, mx]), rl([mx])))
                ch.append(lambda: ts_("dve", sc2.ap, sc.ap, mx.ap[:, 15:16], None, ALU.is_ge, None, [sc, mx, sc2], [sc2]))
                ch.append(lambda: cp("dve", selb.ap, sc2.ap, [sc2], [selb]))

                def x2(selT4=selT4):
                    pb = bankbf(6)
                    tr(pb[0:64, 0:128], selb.ap, ident.ap, [selb, ident], [banks[6]])
                    cp("dve", selT4.ap[0:64, 0:128], pb[0:64, 0:128], [banks[6], selT4], [selT4])
                X2[0] = x2

            def drain_chain(n=None):
                k = 0
                while CHAIN and (n is None or k < n):
                    CHAIN.pop(0)()
                    k += 1

            def stage_y(tt):
                Q = qT3[:, :, tt * 128:(tt + 1) * 128]
                oc = oc_t[tt % 2]
                oc3 = r3(oc.ap, 4)
                rdc = rdc_t[tt % 2]
                selT4 = selT4_t[tt % 2]
                k0 = max(0, tt - 4)
                its = [("s", kt) for kt in range(tt + 1)] + [("w", kt) for kt in range(k0, tt + 1)]

                def emit_s(j, tt=tt, Q=Q, its=its, k0=k0):
                    kind, kt = its[j]
                    q = psi0 + j
                    Sb = banks[SBK[q % 3]]
                    P_ = Ps[q % 4]
                    diag = kt == tt
                    if kind == "s":
                        mm(r3(Sb.ap, 4), ksT.ap[:, kt * 128:(kt + 1) * 128], Q, True, not diag, [ksT, qT], [Sb])
                        if diag:
                            mm(Sb.ap, ident.ap, tri4.ap, False, True, [ident, tri4], [Sb])
                        mreg = banks[7].ap[:, (q % 4) * 128:(q % 4) * 128 + 128]
                        mm(mreg, ee.ap[0:64, kt * 128:(kt + 1) * 128], selT4.ap[0:64, 0:128], True, True, [ee, selT4], [banks[7]])
                        act(P_.ap, Sb.ap, AF.Exp, [Sb], [P_], scale=SCALE)
                        mk = msk_t[q % 4]
                        cp("act", mk.ap, mreg, [banks[7]], [mk])
                        tt_("dve", r3(P_.ap, 4), r3(P_.ap, 4), mk.ap.unsqueeze(1).broadcast_to([128, 4, 128]), ALU.mult,
                            [P_, mk], [P_])
                    else:
                        low = kt == tt - 4
                        mm(r3(Sb.ap, 4), kwT.ap[:, kt * 128:(kt + 1) * 128], Q, True, not (diag or low), [kwT, qT], [Sb])
                        if diag:
                            mm(Sb.ap, ident.ap, tri4.ap, False, True, [ident, tri4], [Sb])
                        if low:
                            mm(Sb.ap, ident.ap, tri24.ap, False, True, [ident, tri24], [Sb])
                        act(P_.ap, Sb.ap, AF.Exp, [Sb], [P_], scale=SCALE)

                def emit_av(j, tt=tt, its=its, k0=k0):
                    kind, kt = its[j]
                    P_ = Ps[(psi0 + j) % 4]
                    for r in range(4):
                        bo = 2 + r
                        if kind == "s":
                            mm(banks[bo].ap[:, 0:129], P_.ap[:, r * 128:(r + 1) * 128], vsa3[:, kt, :],
                               kt == 0, kt == tt, [P_, vsa], [banks[bo]])
                        else:
                            mm(banks[bo].ap[:, 0:129], P_.ap[:, r * 128:(r + 1) * 128], vwa3[:, kt, :],
                               kt == k0, kt == tt, [P_, vwa], [banks[bo]])
                    if kind == "s" and kt == tt:
                        for r in range(4):
                            cp("act" if r % 2 == 0 else "dve", osl3[:, r, :], banks[2 + r].ap[:, 0:129], [banks[2 + r], osl], [osl])

                psi0 = PSI[0]
                nn = len(its)
                for j in range(min(2, nn)):
                    emit_s(j)
                for j in range(nn):
                    if j + 2 < nn:
                        emit_s(j + 2)
                    emit_av(j)
                    drain_chain(1)
                PSI[0] += nn
                if YTR[0] is not None:
                    YTR[0]()
                    YTR[0] = None
                drain_chain()
                if X2[0] is not None:
                    X2[0]()
                    X2[0] = None
                cp("dve", rd3[:, 0, :], rdc.ap, [rdc, rd], [rd])
                for r in range(4):
                    cp("act" if r % 2 == 0 else "dve", owi3[:, r, :], banks[2 + r].ap[:, 0:129], [banks[2 + r], owi], [owi])
                recip(rd3[:, 1, :], osl3[:, :, 128], [osl, rd], [rd])
                recip(rd3[:, 2, :], owi3[:, :, 128], [owi, rd], [rd])
                tt_("dve", cf3, rd3, r3(gat3[:, tt, :], 4).rearrange("p h c -> p c h"), ALU.mult, [rd, gat], [cf])
                bc = lambda i: cf3[:, i, :].unsqueeze(2).broadcast_to([128, 4, 128])
                tt_("pool", r3(o1.ap, 4), oc3[:, :, 0:128], bc(0), ALU.mult, [oc, cf], [o1])
                tt_("pool", r3(o2.ap, 4), osl3[:, :, 0:128], bc(1), ALU.mult, [osl, cf], [o2])
                tt_("pool", r3(o3.ap, 4), owi3[:, :, 0:128], bc(2), ALU.mult, [owi, cf], [o3])
                tt_("pool", o1.ap, o1.ap, o2.ap, ALU.add, [o1, o2], [o1])
                tt_("pool", o1.ap, o1.ap, o3.ap, ALU.add, [o1, o3], [o1])
                tt_("pool", o2.ap, o1.ap, o1.ap, ALU.mult, [o1, o2], [o2])
                rsum(ssq.ap, r3(o2.ap, 4), [o2], [ssq])
                rstd_lnexp(ssq.ap, ssq.ap, 1.0 / 128, [ssq], [ssq], l4)
                tt_("pool", r3(o2.ap, 4), r3(o1.ap, 4), ssq.ap.unsqueeze(2).broadcast_to([128, 4, 128]), ALU.mult, [o1, ssq], [o2])
                yb = yb_t[tt % 2]
                tt_("pool", yb.ap, o2.ap, gon.ap, ALU.mult, [o2, gon], [yb])

                def ytr(tt=tt, yb=yb):
                    pb = bankbf(6)
                    for r in range(4):
                        tr(pb[:, r * 128:(r + 1) * 128], yb.ap[:, r * 128:(r + 1) * 128], ident.ap, [yb, ident], [banks[6]])
                    ys = ystage[(tt // 4) % 2]
                    ys3 = r3(ys.ap, 4)
                    cp("act", ys3[:, :, (tt % 4) * 128:(tt % 4) * 128 + 128], r3(pb[:, 0:512], 4), [banks[6]], [ys])
                    if tt % 4 == 3:
                        tg = tt // 4
                        dma("sp", yT_v[:, 4 * g:4 * g + 4, tg * 512:(tg + 1) * 512], ys3, [ys], [dres("yT", g, tg)])
                YTR[0] = ytr


            stage_x(0)
            drain_chain()
            X2[0]()
            X2[0] = None
            for tt in range(NT):
                if tt + 1 < NT:
                    stage_x(tt + 1)
                stage_y(tt)
            YTR[0]()
            YTR[0] = None

            phase_reset()
            qs = AR.tile(4 * S)
            ks_ = AR.tile(4 * S)
            qs3 = r3(qs.ap, 4)
            ks3 = r3(ks_.ap, 4)
            fall = lambda c0, c1: [dres("featT", g, ch, tg) for ch in range(c0, c1) for tg in range(8)]
            dma("sp", qs3, featT_d[g, 1:5, :, :].rearrange("i p t -> p i t"), fall(1, 5), [qs])
            dma("sp", ks3, featT_d[g, 5:9, :, :].rearrange("i p t -> p i t"), fall(5, 9), [ks_])
            vsbt = AR.tile(NT * 512)
            vsb3 = r3(vsbt.ap, NT)
            dma("sp", vsb3, vsb_d[g].rearrange("(n p) c -> p n c", p=128), [dres("vsb", g, tt) for tt in range(NT)], [vsbt])
            sbm = AR.tile(2048)
            sbm3 = r3(sbm.ap, 4)
            dma("pool", sbm.ap, sbm_in[:, :], [], [sbm])
            ust = AR.tile(128)
            ule = AR.tile(128)
            onesb = AR.tile(128)
            dma("pool", ust.ap, ust_in[:, :], [], [ust])
            dma("pool", ule.ap, ule_in[:, :], [], [ule])
            mset("dve", onesb.ap, 1.0, [onesb])
            sbon = AR.tile(8, F32)
            dma("sp", sbon.ap, sbon_in[:, :], [], [sbon])
            LA = 4
            NB = 6
            e_t = [AR.tile(512, F32) for _ in range(2)]
            sp_t = [AR.tile(512, F32) for _ in range(2)]
            spb_t = [AR.tile(512) for _ in range(NB)]
            a1_t = [AR.tile(512, F32) for _ in range(NB)]
            at_t = [AR.tile(512) for _ in range(NB)]
            sqb = AR.tile(512)
            od = AR.tile(512, F32)
            rst = AR.tile(512, F32)
            lnt = AR.tile(512, F32)
            yst = [AR.tile(512) for _ in range(2)]
            items = []
            for h in range(4):
                for qc in range(8):
                    kts = list(range(4 * qc + 3, -1, -1))
                    for ki_, kt in enumerate(kts):
                        items.append((h, qc, kt, ki_ == 0, ki_ == len(kts) - 1, (h * 8 + qc) % 2))
            n_it = len(items)

            def a_pe(i):
                h, qc, kt, first, last, sidx = items[i]
                zb = banks[i % 2]
                o = kt - 4 * qc
                mm(zb.ap, ks3[:, h, kt * 128:(kt + 1) * 128], qs3[:, h, qc * 512:(qc + 1) * 512], True, o < 0,
                   [ks_, qs], [zb])
                if o >= 0:
                    mm(zb.ap, ident.ap, sbm3[:, o, :], False, True, [ident, sbm], [zb])

            def a_act(i):
                zb = banks[i % 2]
                e_ = e_t[i % 2]
                sp_ = sp_t[i % 2]
                act(e_.ap, zb.ap, AF.Exp, [zb], [e_])
                act(sp_.ap, e_.ap, AF.Ln, [e_, one_t], [sp_], bias=one_t.ap)

            def a_post(i):
                zb = banks[i % 2]
                sp_ = sp_t[i % 2]
                spb = spb_t[i % NB]
                a1 = a1_t[i % NB]
                tt_("dve", a1.ap, zb.ap, sp_.ap, ALU.subtract, [zb, sp_], [a1])
                cp("pool", spb.ap, sp_.ap, [sp_], [spb])

            def b1_pe(i):
                h, qc, kt, first, last, sidx = items[i]
                Cb = banks[2 + sidx]
                mm(Cb.ap, ust.ap, spb_t[i % NB].ap, first, True, [ust, spb_t[i % NB]], [Cb])

            def b1_dve(i):
                h, qc, kt, first, last, sidx = items[i]
                Cb = banks[2 + sidx]
                a1 = a1_t[i % NB]
                tt_("dve", a1.ap, a1.ap, Cb.ap, ALU.subtract, [a1, Cb], [a1])

            def b1_act(i):
                a1 = a1_t[i % NB]
                at = at_t[i % NB]
                act(at.ap, a1.ap, AF.Exp, [a1], [at])

            def stage_b2(i):
                h, qc, kt, first, last, sidx = items[i]
                Cb = banks[2 + sidx]
                spb = spb_t[i % NB]
                mm(Cb.ap, ule.ap, spb.ap, False, True, [ule, spb], [Cb])

            def stage_av(i):
                h, qc, kt, first, last, sidx = items[i]
                Db = banks[4 + sidx]
                at = at_t[i % NB]
                mm(Db.ap, vsb3[:, kt, h * 128:(h + 1) * 128], at.ap, first, last, [vsbt, at], [Db])
                if last:
                    cp("act", od.ap, Db.ap, [Db], [od])
                    tt_("dve", sqb.ap, od.ap, od.ap, ALU.mult, [od], [sqb])
                    mm(banks[6].ap, onesb.ap, sqb.ap, True, True, [onesb, sqb], [banks[6]])
                    act(lnt.ap, banks[6].ap, AF.Ln, [banks[6]], [lnt], bias=eps_t.ap, scale=1.0 / 128)
                    act(rst.ap, lnt.ap, AF.Exp, [lnt], [rst], scale=-0.5)
                    ys = yst[(h * 8 + qc) % 2]
                    stt("dve", ys.ap, od.ap, sbon.ap[:, 4 * g + h:4 * g + h + 1], rst.ap, ALU.mult, ALU.mult, [od, sbon, rst], [ys])
                    dma("sp", yT_d[8 + 4 * g + h, :, qc * 512:(qc + 1) * 512], ys.ap, [ys], [dres("yTsb", g, h, qc)])

            for i in range(LA):
                a_pe(i)
                a_act(i)
                a_post(i)
            for i in range(n_it + 1):
                if i < n_it:
                    b1_pe(i)
                    b1_dve(i)
                if i + LA < n_it:
                    a_pe(i + LA)
                    a_act(i + LA)
                    a_post(i + LA)
                if i < n_it:
                    b1_act(i)
                if 1 <= i:
                    stage_av(i - 1)
                if i < n_it:
                    stage_b2(i)

        phase_reset()
        wo = AR.tile(16 * D)
        wo3 = r3(wo.ap, 16)
        wov = w_out.rearrange("(c p) f -> p c f", p=128)
        for q in range(4):
            dma("pool", wo3[:, q * 4:(q + 1) * 4, :], wov[:, q * 4:(q + 1) * 4, :], [wo] if q else [], [wo])
        yall = [dres("yT", g, tg) for g in range(2) for tg in range(8)] + \
               [dres("yTsb", g, h, qc) for g in range(2) for h in range(4) for qc in range(8)]
        ybA = [AR.tile(16 * 512) for _ in range(2)]
        ybB = AR.tile(16 * 512)
        xa = [AR.tile(D, F32) for _ in range(2)]
        xb_ = AR.tile(D, F32)
        x2t = [AR.tile(D, F32) for _ in range(2)]
        yT_v = yT_d.rearrange("c p t -> p c t")
        for tg in range(4):
            ya = ybA[tg % 2]
            ya3 = r3(ya.ap, 16)
            dma("sp", ya3, yT_v[:, :, tg * 512:(tg + 1) * 512], yall, [ya])
            dma("sp", r3(ybB.ap, 16), yT_v[:, :, 2048 + tg * 512:2048 + (tg + 1) * 512], yall, [ybB])
            ts_("dve", ya.ap, ya.ap, sel_t.ap[:, 0:1], None, ALU.mult, None, [ya, sel_t], [ya])
            stt("dve", ya.ap, ybB.ap, sel_t.ap[:, 1:2], ya.ap, ALU.mult, ALU.add, [ybB, sel_t, ya], [ya])
            for t4 in range(4):
                tl = tg * 4 + t4
                xt = xa[tl % 2]
                dma("sp", xt.ap, x1_d[tl * 128:(tl + 1) * 128, :], [dres("x1", tl, q) for q in range(4)], [xt])
                dma("sp", xb_.ap, x1_d[2048 + tl * 128:2048 + (tl + 1) * 128, :], [dres("x1", 16 + tl, q) for q in range(4)], [xb_])
                ts_("dve", xt.ap, xt.ap, sel_t.ap[:, 0:1], None, ALU.mult, None, [xt, sel_t], [xt])
                stt("dve", xt.ap, xb_.ap, sel_t.ap[:, 1:2], xt.ap, ALU.mult, ALU.add, [xb_, sel_t, xt], [xt])
                x2 = x2t[tl % 2]
                for dmc in range(4):
                    b = (tl * 4 + dmc) % 8
                    for c in range(16):
                        mm(banks[b].ap, ya3[:, c, t4 * 128:(t4 + 1) * 128], wo3[:, c, dmc * 512:(dmc + 1) * 512], c == 0, c == 15,
                           [ya, wo], [banks[b]])
                    tt_("dve", x2.ap[:, dmc * 512:(dmc + 1) * 512], banks[b].ap, xt.ap[:, dmc * 512:(dmc + 1) * 512], ALU.add,
                        [banks[b], xt], [x2])
                dma("sp", x2_d[tl * 128:(tl + 1) * 128, :], x2.ap, [x2], [dres("x2", tl)])

        ffn_phase("f2", x2_d, lambda tt: [dres("x2", tt)], 2048, ffw[2], out_d, lambda tt, dmc: dres("out", tt, dmc))

        S_.emit(nc, sems, dsems)
    return nc


def _consts():
    c = {}
    c["ident"] = np.eye(128, dtype=np.float32)
    c["invf"] = (10000.0 ** (-np.arange(0, 128, 2, dtype=np.float32) / 128)).astype(np.float32).reshape(1, 64)
    k = np.arange(128)[:, None]
    t = np.arange(128)[None, :]
    tri = np.where(k > t, NEG, 0.0).astype(np.float32)
    c["tri4"] = np.tile(tri, (1, 4))
    tri2 = np.where(k <= t, NEG, 0.0).astype(np.float32)
    c["tri24"] = np.tile(tri2, (1, 4))
    strict = np.where(k >= t, NEG, 0.0).astype(np.float32)
    sbm = np.zeros((128, 4, 4, 128), np.float32)
    for o in range(4):
        for j in range(4):
            if j < o:
                sbm[:, o, j, :] = NEG
            elif j == o:
                sbm[:, o, j, :] = strict
    c["sbm"] = sbm.reshape(128, 2048)
    n = np.arange(256)[:, None]
    tt = np.arange(S)[None, :]
    c["cbt"] = np.where((16 * n + 31 > tt) | (n >= 255), NEG, 0.0).astype(np.float32)
    mm_ = np.zeros((256, 65), np.float32)
    for blk in range(64):
        for nn in range(4 * blk - 1, 4 * blk + 4):
            if 0 <= nn < 255:
                mm_[nn, blk] = 1.0
    mm_[:255, 64] = 1.0
    c["mmat"] = mm_
    tq = np.arange(S)[:, None]
    blk = np.arange(64)[None, :]
    cur = tq // 64
    forced = (blk == 0) | (blk == cur) | (blk == cur - 1)
    causal = blk * 64 <= tq
    c["atab"] = np.where(causal & ~forced, 1.0, 0.0).astype(np.float32)
    c["btab"] = np.where(causal, np.where(forced, 1e9, 0.0), -1e30).astype(np.float32)
    key = np.arange(S)[None, :]
    c["ee"] = np.where(key // 64 == np.arange(64)[:, None], 1.0, 0.0).astype(np.float32)
    j = np.arange(128)[:, None]
    s = np.arange(128)[None, :]
    c["ust"] = (j > s).astype(np.float32)
    c["ule"] = (j <= s).astype(np.float32)
    return c


def _w_in_groups(w_in):
    outs = []
    for g in range(2):
        cols = []
        cols += list(range(g * 512, g * 512 + 512))
        cols += list(range(1024 + g * 128, 1024 + g * 128 + 128))
        cols += list(range(1536 + g * 128, 1536 + g * 128 + 128))
        cols += list(range(2048 + g * 128, 2048 + g * 128 + 128))
        cols += list(range(1792 + g * 128, 1792 + g * 128 + 128))
        cols += list(range(2304 + g * 128, 2304 + g * 128 + 128))
        cols += list(range(2560 + g * 12, 2560 + g * 12 + 12))
        cols += list(range(4632 + g * 512, 4632 + g * 512 + 512))
        cols += list(range(1280 + g * 128, 1280 + g * 128 + 128))
        cols += list(range(2584 + g * 512, 2584 + g * 512 + 512))
        cols += list(range(3608 + g * 512, 3608 + g * 512 + 512))
        assert len(cols) == 2828
        outs.append(w_in[:, cols])
    return np.ascontiguousarray(np.stack(outs, 0))


_NC_CACHE = {}


def make_in_maps(inputs):
    f = lambda a: np.ascontiguousarray(np.asarray(a, dtype=np.float32))
    x = f(inputs["x"])
    pos = np.asarray(inputs["positions"]).astype(np.int32)
    shared = dict(_consts())
    for i in (1, 2):
        shared[f"f{i}_wg"] = f(inputs[f"ffn{i}_w_gate"][0])
        shared[f"f{i}_wu"] = f(inputs[f"ffn{i}_w_up"][0])
        shared[f"f{i}_wd"] = f(inputs[f"ffn{i}_w_down"][0])
        shared[f"f{i}_norm"] = f(inputs[f"ffn{i}_norm"]).reshape(1, D)
    shared["mix_norm"] = f(inputs["mix_norm"]).reshape(1, D)
    shared["w_in_g"] = _w_in_groups(f(inputs["w_in"][0]))
    qn = f(inputs["nsa_q_norm"]).reshape(128)
    shared["g7"] = np.concatenate([qn, qn, qn, qn, np.ones(128, np.float32), f(inputs["nsa_k_slc_norm"]).reshape(128),
                                   f(inputs["nsa_k_win_norm"]).reshape(128)]).reshape(1, 896)
    shared["kcn"] = f(inputs["nsa_k_cmp_norm"]).reshape(1, 128)
    shared["nsaon"] = f(inputs["nsa_out_norm"]).reshape(2, 512)
    shared["sbon"] = np.ascontiguousarray(f(inputs["sb_out_norm"]).reshape(8, 128).T)
    shared["ck_w1"] = f(inputs["cmp_k_w1"][0])
    shared["cv_w1"] = f(inputs["cmp_v_w1"][0])
    shared["ck_w2"] = f(inputs["cmp_k_w2"][0])
    shared["cv_w2"] = f(inputs["cmp_v_w2"][0])
    shared["ck_pos"] = f(inputs["cmp_k_pos"][0])
    shared["cv_pos"] = f(inputs["cmp_v_pos"][0])
    shared["w_out"] = f(inputs["w_out"][0])
    in_maps = []
    for c in range(8):
        b, r = c // 2, c % 2
        m = dict(shared)
        m["x_b"] = np.ascontiguousarray(x[b])
        m["pos"] = np.ascontiguousarray(pos[b].reshape(NT, 128).T)
        sel = np.zeros((128, 2), np.float32)
        sel[:, r] = 1.0
        m["sel"] = sel
        in_maps.append(m)
    return in_maps


def kernel(**inputs):
    in_maps = make_in_maps(inputs)
    if "nc" not in _NC_CACHE:
        _NC_CACHE["nc"] = build_program()
    nc = _NC_CACHE["nc"]
    res = run_bass_kernel_spmd(nc, in_maps, core_ids=list(range(8)))
    out = np.zeros((4, S, D), np.float32)
    for c in range(8):
        b, r = c // 2, c % 2
        out[b, r * 2048:(r + 1) * 2048, :] = res.results[c]["out"]
    _NC_CACHE["last"] = res
    return out
```

```python
import numpy as np
import concourse.bass as bass
import concourse.mybir as mybir
from concourse.bass_utils import run_bass_kernel_spmd

F32 = mybir.dt.float32
BF16 = mybir.dt.bfloat16
I32 = mybir.dt.int32
AF = mybir.ActivationFunctionType
ALU = mybir.AluOpType
AX = mybir.AxisListType

D = 2048
DC = 16
FF = 5632
FCN = 44
S = 4096
NT = 32
HD = 128
NEG = -30000.0
SCALE = 128 ** -0.5
EPS = 1e-6
DEBUG = False
SAME_ENGINE_SYNC = True
KD = 8


PHASE_MARKS = []


class Res:
    __slots__ = ("w", "r")

    def __init__(self):
        self.w = None
        self.r = []


class Op:
    __slots__ = ("eng", "fn", "deps", "dma", "inc", "tok", "pre")

    def __init__(self, eng, fn, dma):
        self.eng = eng
        self.fn = fn
        self.dma = dma
        self.deps = []
        self.inc = False
        self.tok = None
        self.pre = None


class T:
    __slots__ = ("ap", "res")

    def __init__(self, ap, res=None):
        self.ap = ap
        self.res = res if res is not None else Res()


ENGS = ["pe", "act", "dve", "pool", "sp"]


class Sched:
    def __init__(self):
        self.ops = {e: [] for e in ENGS}
        self.dmas = {"sp": [], "pool": []}
        self.pending = {e: [] for e in ENGS}

    def add(self, eng, fn, reads=(), writes=(), dma=False):
        op = Op(eng, fn, dma)
        deps = {}
        for r in reads:
            if r.w is not None:
                deps[id(r.w)] = r.w
        for w in writes:
            if w.w is not None:
                deps[id(w.w)] = w.w
            for q in w.r:
                deps[id(q)] = q
        for p in self.pending[eng]:
            deps[id(p)] = p
        self.pending[eng] = []
        for r in reads:
            r.r.append(op)
        for w in writes:
            w.w = op
            w.r = []
        for d in deps.values():
            if d is op:
                continue
            if (not d.dma) and (not dma) and d.eng == eng:
                if eng == "pe" or not SAME_ENGINE_SYNC:
                    continue
            op.deps.append(d)
            if not d.dma:
                d.inc = True
        self.ops[eng].append(op)
        if dma:
            self.dmas[eng].append(op)
        return op

    def barrier(self):
        lst = []
        for e in ENGS:
            for op in reversed(self.ops[e]):
                if not op.dma:
                    lst.append(op)
                    break
        for q in ("sp", "pool"):
            lst.extend(self.dmas[q][-KD:])
        for e in ENGS:
            self.pending[e] = list(lst)

    def emit(self, nc, sems, dsems):
        for e in ENGS:
            k = 0
            for op in self.ops[e]:
                if (not op.dma) and op.inc:
                    k += 1
                    op.tok = (sems[e], k)
        alld = []
        for q in ("sp", "pool"):
            for i, op in enumerate(self.dmas[q]):
                op.tok = (dsems[q][i % KD], 16 * (i // KD + 1))
                if i >= KD:
                    op.pre = (dsems[q][i % KD], 16 * (i // KD))
            alld.extend(self.dmas[q][-KD:])
        ops = self.ops

        def run(e, eng):
            seen = {}
            for op in ops[e]:
                waits = [d.tok for d in op.deps]
                if op.pre is not None:
                    waits.append(op.pre)
                for sem, val in waits:
                    if seen.get(sem.num, 0) < val:
                        eng.wait_ge(sem, val)
                        seen[sem.num] = val
                ins = op.fn(eng)
                if op.dma:
                    ins.then_inc(op.tok[0], 16)
                elif op.inc:
                    ins.then_inc(op.tok[0], 1)
            if e == "sp":
                for d in alld:
                    sem, val = d.tok
                    if seen.get(sem.num, 0) < val:
                        eng.wait_ge(sem, val)
                        seen[sem.num] = val

        with nc.Block() as block:
            @block.tensor
            def _(t):
                run("pe", t)

            @block.scalar
            def _(s):
                run("act", s)

            @block.vector
            def _(v):
                run("dve", v)

            @block.gpsimd
            def _(g):
                run("pool", g)

            @block.sync
            def _(sy):
                run("sp", sy)


class Arena:
    def __init__(self, ap, ncols):
        self.ap = ap
        self.n = ncols
        self.off = 0

    def alloc(self, cols, dt=BF16):
        sz = 2 if dt == BF16 else 4
        n2 = (cols * sz + 63) // 64 * 32
        assert self.off + n2 <= self.n, ("arena overflow", self.off, n2, self.n)
        a = self.ap[:, self.off:self.off + n2]
        self.off += n2
        if dt == BF16:
            return a[:, 0:cols]
        return a.bitcast(dt)[:, 0:cols]

    def tile(self, cols, dt=BF16, res=None):
        return T(self.alloc(cols, dt), res)


def r3(ap, a):
    return ap.rearrange("p (a b) -> p a b", a=a)


def build_program():
    nc = bass.Bass("TRN2", target_bir_lowering=False)
    S_ = Sched()
    dkind = "ExternalOutput" if DEBUG else "Internal"

    def din(name, shape, dt=F32):
        return nc.dram_tensor(name, list(shape), dt, kind="ExternalInput").ap()

    def dscr(name, shape, dt):
        return nc.dram_tensor(name, list(shape), dt, kind=dkind).ap()

    x_b = din("x_b", [S, D])
    pos_in = din("pos", [128, NT], I32)
    sel_in = din("sel", [128, 2])
    ffw = {}
    for i in (1, 2):
        ffw[i] = (din(f"f{i}_wg", [D, FF]), din(f"f{i}_wu", [D, FF]), din(f"f{i}_wd", [FF, D]), din(f"f{i}_norm", [1, D]))
    mix_norm = din("mix_norm", [1, D])
    w_in_g = din("w_in_g", [2, D, 2828])
    g7_in = din("g7", [1, 896])
    kcn_in = din("kcn", [1, 128])
    nsaon_in = din("nsaon", [2, 512])
    sbon_in = din("sbon", [128, 8])
    cw1 = (din("ck_w1", [4096, 256]), din("cv_w1", [4096, 256]))
    cw2 = (din("ck_w2", [256, 128]), din("cv_w2", [256, 128]))
    cpos = (din("ck_pos", [32, 128]), din("cv_pos", [32, 128]))
    w_out = din("w_out", [D, D])
    ident_in = din("ident", [128, 128])
    invf_in = din("invf", [1, 64])
    tri4_in = din("tri4", [128, 512])
    tri24_in = din("tri24", [128, 512])
    sbm_in = din("sbm", [128, 2048])
    cbt_in = din("cbt", [256, 4096])
    mmat_in = din("mmat", [256, 65])
    atab_in = din("atab", [S, 64])
    btab_in = din("btab", [S, 64])
    ee_in = din("ee", [64, 4096])
    ust_in = din("ust", [128, 128])
    ule_in = din("ule", [128, 128])
    out_d = nc.dram_tensor("out", [2048, D], F32, kind="ExternalOutput").ap()

    x1_d = dscr("x1", [S, D], F32)
    hT_d = dscr("hT", [D, S], BF16)
    ropeT_d = dscr("ropeT", [2, 7, 128, S], BF16)
    featT_d = dscr("featT", [2, 9, 128, S], BF16)
    vsw_d = dscr("vsw", [2, S, 256], BF16)
    gat_d = dscr("gat", [2, S, 12], F32)
    vsb_d = dscr("vsb", [2, S, 512], BF16)
    yT_d = dscr("yT", [16, 128, S], BF16)
    x2_d = dscr("x2", [2048, D], F32)
    cs_d = dscr("cs", [2, 128, NT * 64], F32)
    dbg_kc = dscr("dbg_kc", [2, 128, 256], BF16) if DEBUG else None
    dbg_vc = dscr("dbg_vc", [2, 256, 193], BF16) if DEBUG else None

    dres_map = {}

    def dres(*key):
        r = dres_map.get(key)
        if r is None:
            r = Res()
            dres_map[key] = r
        return r

    ARENA_COLS = 90 * 1024
    import contextlib
    with contextlib.ExitStack() as es:
        arena_t = es.enter_context(nc.sbuf_tensor("arena", [128, ARENA_COLS], BF16))
        banks = []
        for i in range(8):
            pt = es.enter_context(nc.psum_tensor(f"bank{i}", [128, 512], F32))
            banks.append(T(pt[:]))
        sems = {}
        for e in ("pe", "act", "dve", "pool", "sp"):
            sems[e] = es.enter_context(nc.semaphore("s_" + e))
        dsems = {q: [es.enter_context(nc.semaphore(f"d_{q}{i}")) for i in range(KD)] for q in ("sp", "pool")}

        AR = Arena(arena_t[:], ARENA_COLS)

        def rl(ts):
            return [t.res if isinstance(t, T) else t for t in ts]

        def dma(q, out, in_, reads, writes):
            return S_.add(q, lambda e: e.dma_start(out=out, in_=in_), rl(reads), rl(writes), dma=True)

        def mm(out, lhsT, rhs, start, stop, reads, writes):
            return S_.add("pe", lambda e: e.matmul(out, lhsT, rhs, start=start, stop=stop, skip_group_check=True),
                          rl(reads), rl(writes))

        def tr(out, in_, idn, reads, writes):
            return S_.add("pe", lambda e: e.transpose(out, in_, idn), rl(reads), rl(writes))

        def act(out, in_, func, reads, writes, bias=None, scale=1.0, accum=None):
            def f(e):
                kw = {}
                if bias is not None:
                    np_ = out.shape[0]
                    kw["bias"] = bias if bias.shape[0] == np_ else bias[0:np_, :]
                if accum is not None:
                    kw["accum_out"] = accum
                return e.activation(out=out, in_=in_, func=func, scale=scale, **kw)
            return S_.add("act", f, rl(reads), rl(writes))

        def tt_(eng, out, in0, in1, op, reads, writes):
            return S_.add(eng, lambda e: e.tensor_tensor(out=out, in0=in0, in1=in1, op=op), rl(reads), rl(writes))

        def ts_(eng, out, in0, s1, s2, op0, op1, reads, writes):
            if op1 is None:
                return S_.add(eng, lambda e: e.tensor_scalar(out=out, in0=in0, scalar1=s1, scalar2=None, op0=op0),
                              rl(reads), rl(writes))
            return S_.add(eng, lambda e: e.tensor_scalar(out=out, in0=in0, scalar1=s1, scalar2=s2, op0=op0, op1=op1),
                          rl(reads), rl(writes))

        def stt(eng, out, in0, scalar, in1, op0, op1, reads, writes):
            return S_.add(eng, lambda e: e.scalar_tensor_tensor(out=out, in0=in0, scalar=scalar, in1=in1, op0=op0, op1=op1),
                          rl(reads), rl(writes))

        def cp(eng, out, in_, reads, writes):
            if eng == "act":
                return S_.add("act", lambda e: e.copy(out=out, in_=in_), rl(reads), rl(writes))
            return S_.add(eng, lambda e: e.tensor_copy(out=out, in_=in_), rl(reads), rl(writes))

        def mset(eng, ap, val, writes):
            return S_.add(eng, lambda e: e.memset(ap, val), [], rl(writes))

        def rsum(out, in_, reads, writes):
            return S_.add("dve", lambda e: e.reduce_sum(out=out, in_=in_, axis=AX.X), rl(reads), rl(writes))

        def recip(out, in_, reads, writes):
            return S_.add("dve", lambda e: e.reciprocal(out=out, in_=in_), rl(reads), rl(writes))

        def bankbf(b):
            return banks[b].ap.bitcast(BF16)

        ident = AR.tile(128)
        dma("pool", ident.ap, ident_in[:, :], [], [ident])
        eps_t = AR.tile(1, F32)
        one_t = AR.tile(1, F32)
        n2pi_t = AR.tile(1, F32)
        mset("dve", eps_t.ap, EPS, [eps_t])
        mset("dve", one_t.ap, 1.0, [one_t])
        mset("dve", n2pi_t.ap, -6.283185, [n2pi_t])
        sel_t = AR.tile(2, F32)
        dma("sp", sel_t.ap, sel_in[:, :], [], [sel_t])
        PERSIST = AR.off

        def phase_reset():
            S_.barrier()
            AR.off = PERSIST
            PHASE_MARKS.append({e: len(S_.ops[e]) for e in ENGS})

        def rstd_lnexp(out_ap, ss_ap, inv_n, reads, writes, tmp):
            act(tmp.ap, ss_ap, AF.Ln, reads, [tmp], bias=eps_t.ap, scale=inv_n)
            act(out_ap, tmp.ap, AF.Exp, [tmp], writes, scale=-0.5)

        def norm_transpose(src_ap, src_res, xt, gt, junk, xn, ss, rt, dst_ap_fn, dst_res, blend=None):
            if blend is None:
                dma("sp", xt.ap, src_ap, src_res, [xt])
            else:
                blend()
            mset("dve", ss.ap, 0.0, [ss])
            act(junk.ap, xt.ap, AF.Square, [xt, ss], [junk, ss], accum=ss.ap)
            act(rt.ap, ss.ap, AF.Sqrt, [ss], [rt], bias=eps_t.ap, scale=1.0 / D)
            recip(rt.ap, rt.ap, [rt], [rt])
            stt("dve", xn.ap, xt.ap, rt.ap, gt.ap, ALU.mult, ALU.mult, [xt, rt, gt], [xn])
            for hb in range(2):
                b = 6 + hb
                pb = bankbf(b)
                for j in range(8):
                    dc = hb * 8 + j
                    tr(pb[:, j * 128:(j + 1) * 128], xn.ap[:, dc * 128:(dc + 1) * 128], ident.ap, [xn, ident], [banks[b]])
                cp("act" if hb == 0 else "dve", dst_ap_fn(hb * 8, 8), r3(pb[:, 0:1024], 8), [banks[b]], dst_res)

        def ffn_phase(tag, x_src, src_res_fn, ntok, wts, dst_ap, dst_res_fn, then_norm=None):
            wg_d, wu_d, wd_d, nrm_d = wts
            phase_reset()
            xnT = AR.alloc(16 * 1024)
            xnT3 = r3(xnT, 16)
            xn_res = [[Res() for _ in range(2)] for _ in range(8)]
            actT = AR.alloc(44 * 1024)
            actT3 = r3(actT, 44)
            act_res = [[Res() for _ in range(2)] for _ in range(44)]
            slots = [AR.tile(4096) for _ in range(4)]
            wdb = [AR.tile(2048) for _ in range(2)]
            silu_t = [AR.tile(512, F32) for _ in range(2)]
            NXR = 6
            xres_t = [AR.tile(512, F32) for _ in range(NXR)]
            xrc = [0]

            def load_res(tg, dmc, t8):
                row0 = tg * 1024 + t8 * 128
                xr_ = xres_t[(xrc[0] + t8) % NXR]
                dma("sp", xr_.ap, x_src[row0:row0 + 128, dmc * 512:(dmc + 1) * 512], src_res_fn(row0 // 128), [xr_])
            ss = AR.tile(1, F32)
            rt = AR.tile(1, F32)
            xtA = T(slots[0].ap.bitcast(F32), slots[0].res)
            xtB = T(slots[1].ap.bitcast(F32), slots[1].res)
            gt = T(slots[2].ap.bitcast(F32), slots[2].res)
            xn = T(slots[3].ap[:, 0:2048], slots[3].res)
            junk = T(slots[3].ap[:, 2048:4096], slots[3].res)
            wg_v = wg_d.rearrange("(c p) f -> p c f", p=128)
            wu_v = wu_d.rearrange("(c p) f -> p c f", p=128)
            ngrp = ntok // 1024
            for tg in range(ngrp):
                dma("sp", gt.ap, nrm_d[0:1, :].partition_broadcast(128), [], [gt])
                for t8 in range(8):
                    row0 = tg * 1024 + t8 * 128
                    xt = xtA if t8 % 2 == 0 else xtB

                    def dst_fn(dc0, n, t8=t8):
                        return xnT3[:, dc0:dc0 + n, t8 * 128:(t8 + 1) * 128]
                    norm_transpose(x_src[row0:row0 + 128, :], src_res_fn(row0 // 128), xt, gt, junk, xn, ss, rt,
                                   dst_fn, [xn_res[t8][0], xn_res[t8][1]])
                for fp in range(22):
                    wgt = slots[(fp % 2) * 2]
                    wut = slots[(fp % 2) * 2 + 1]
                    dma("pool", r3(wgt.ap, 16), wg_v[:, :, fp * 256:(fp + 1) * 256], [], [wgt])
                    dma("pool", r3(wut.ap, 16), wu_v[:, :, fp * 256:(fp + 1) * 256], [], [wut])
                    for j in range(2):
                        fc = fp * 2 + j
                        bs = 4 * (fc % 2)
                        for (wt, boff) in ((wgt, 0), (wut, 2)):
                            w3 = r3(wt.ap, 16)
                            for dc in range(16):
                                for hf in range(2):
                                    mm(banks[bs + boff + hf].ap, w3[:, dc, j * 128:(j + 1) * 128],
                                       xnT3[:, dc, hf * 512:(hf + 1) * 512], dc == 0, dc == 15,
                                       [wt] + [xn_res[hf * 4 + q][dc // 8] for q in range(4)], [banks[bs + boff + hf]])
                        for hf in range(2):
                            st_ = silu_t[hf]
                            act(st_.ap, banks[bs + hf].ap, AF.Silu, [banks[bs + hf]], [st_])
                            tt_("dve", actT3[:, fc, hf * 512:(hf + 1) * 512], st_.ap, banks[bs + 2 + hf].ap, ALU.mult,
                                [st_, banks[bs + 2 + hf]], [act_res[fc][hf]])
                for dmc in range(4):
                    for t8 in range(NXR):
                        load_res(tg, dmc, t8)
                    for fq in range(11):
                        wdt = wdb[fq % 2]
                        dma("pool", r3(wdt.ap, 4),
                            wd_d[fq * 512:(fq + 1) * 512, dmc * 512:(dmc + 1) * 512].rearrange("(j p) c -> p j c", p=128),
                            [], [wdt])
                        w3 = r3(wdt.ap, 4)
                        for j in range(4):
                            fc = fq * 4 + j
                            for t8 in range(8):
                                mm(banks[t8].ap, actT3[:, fc, t8 * 128:(t8 + 1) * 128], w3[:, j, :], fc == 0, fc == 43,
                                   [wdt, act_res[fc][t8 // 4]], [banks[t8]])
                    for t8 in range(8):
                        row0 = tg * 1024 + t8 * 128
                        xr_ = xres_t[(xrc[0] + t8) % NXR]
                        stt("dve", xr_.ap, banks[t8].ap, 0.5, xr_.ap, ALU.mult, ALU.add, [banks[t8], xr_], [xr_])
                        dma("sp", dst_ap[row0:row0 + 128, dmc * 512:(dmc + 1) * 512], xr_.ap, [xr_], [dst_res_fn(row0 // 128, dmc)])
                        if t8 + NXR < 8:
                            load_res(tg, dmc, t8 + NXR)
                    xrc[0] += 8

        ffn_phase("f1", x_b, lambda tt: [], S, ffw[1], x1_d, lambda tt, dmc: dres("x1", tt, dmc))

        phase_reset()
        gt = AR.tile(2048, F32)
        dma("sp", gt.ap, mix_norm[0:1, :].partition_broadcast(128), [], [gt])
        xts = [AR.tile(2048, F32) for _ in range(2)]
        xn = AR.tile(2048)
        junk = AR.tile(2048)
        ss = AR.tile(1, F32)
        rt = AR.tile(1, F32)
        xn_l = [xn, AR.tile(2048)]
        junk_l = [junk, AR.tile(2048)]
        ss_l = [ss, AR.tile(1, F32)]
        rt_l = [rt, AR.tile(1, F32)]
        hb_t = [AR.tile(16 * 512) for _ in range(2)]
        hT_v = hT_d.rearrange("(c p) t -> p c t", p=128)
        for tg in range(8):
            hb = hb_t[tg % 2]
            hb3 = r3(hb.ap, 16)
            for t4 in range(4):
                tt = tg * 4 + t4

                def dst_fn(dc0, n, t4=t4, hb3=hb3):
                    return hb3[:, dc0:dc0 + n, t4 * 128:(t4 + 1) * 128]
                norm_transpose(x1_d[tt * 128:(tt + 1) * 128, :], [dres("x1", tt, q) for q in range(4)], xts[tt % 2], gt,
                               junk_l[tt % 2], xn_l[tt % 2], ss_l[tt % 2], rt_l[tt % 2], dst_fn, [hb])
            dma("sp", hT_v[:, :, tg * 512:(tg + 1) * 512], hb3, [hb], [dres("hT", tg)])

        phase_reset()
        invf = AR.tile(64, F32)
        dma("sp", invf.ap, invf_in[0:1, :].partition_broadcast(128), [], [invf])
        posi = AR.tile(NT, I32)
        dma("sp", posi.ap, pos_in[:, :], [], [posi])
        posf = AR.tile(NT, F32)
        cp("dve", posf.ap, posi.ap, [posi], [posf])
        ang0 = AR.tile(NT * 64, F32)
        ta = AR.tile(NT * 64, F32)
        tb = AR.tile(NT * 64, F32)
        ki = AR.tile(NT * 64, I32)
        tab = AR.tile(NT * 64, F32)
        tt_("dve", r3(ang0.ap, NT), posf.ap.unsqueeze(2).broadcast_to([128, NT, 64]),
            invf.ap.unsqueeze(1).broadcast_to([128, NT, 64]), ALU.mult, [posf, invf], [ang0])
        for ci, shift in ((0, 0.0), (1, 1.5707963267948966)):
            ts_("dve", ta.ap, ang0.ap, shift, None, ALU.add, None, [ang0, ta], [ta])
            ts_("dve", tb.ap, ta.ap, 0.15915494309189535, None, ALU.mult, None, [ta, tb], [tb])
            cp("dve", ki.ap, tb.ap, [tb, ki], [ki])
            cp("dve", tb.ap, ki.ap, [ki], [tb])
            stt("dve", ta.ap, tb.ap, -6.28125, ta.ap, ALU.mult, ALU.add, [tb, ta], [ta])
            stt("dve", ta.ap, tb.ap, -0.0019353071795864769, ta.ap, ALU.mult, ALU.add, [tb, ta], [ta])
            ts_("dve", tb.ap, ta.ap, 3.14159265, None, ALU.is_gt, None, [ta], [tb])
            stt("dve", ta.ap, tb.ap, -6.283185307179586, ta.ap, ALU.mult, ALU.add, [tb, ta], [ta])
            ts_("dve", ta.ap, ta.ap, 3.141592, -3.141592, ALU.min, ALU.max, [ta], [ta])
            act(tab.ap, ta.ap, AF.Sin, [ta, tab], [tab], scale=1.0)
            dma("sp", cs_d[ci, :, :], tab.ap, [tab], [dres("cs", ci)])

        for g in range(2):
            phase_reset()
            W = AR.tile(16 * 2828)
            W3 = r3(W.ap, 16)
            wv = w_in_g[g].rearrange("(c p) f -> p c f", p=128)
            dma("pool", W3[:, 0:8, :], wv[:, 0:8, :], [], [W])
            dma("pool", W3[:, 8:16, :], wv[:, 8:16, :], [W], [W])
            g7 = AR.tile(896, F32)
            dma("sp", g7.ap, g7_in[0:1, :].partition_broadcast(128), [], [g7])
            cosT = AR.tile(NT * 64, F32)
            sinT = AR.tile(NT * 64, F32)
            cos3 = r3(cosT.ap, NT)
            sin3 = r3(sinT.ap, NT)
            dma("sp", sinT.ap, cs_d[0, :, :], [dres("cs", 0)], [sinT])
            dma("sp", cosT.ap, cs_d[1, :, :], [dres("cs", 1)], [cosT])
            hbuf = [AR.tile(16 * 512) for _ in range(2)]
            fstage = [AR.tile(512) for _ in range(2)]
            xr = AR.tile(896, F32)
            sq = AR.tile(896, F32)
            ss7 = AR.tile(7, F32)
            l7 = AR.tile(7, F32)
            t1 = AR.tile(448, F32)
            t2 = AR.tile(448, F32)
            t3 = AR.tile(448, F32)
            t4t = AR.tile(448, F32)
            ro = AR.tile(896)
            rstage = [AR.tile(7 * 512) for _ in range(1)]
            vstage = [AR.tile(256) for _ in range(2)]
            gexp = AR.tile(12, F32)
            gstage = [AR.tile(12, F32) for _ in range(2)]
            cstage = [AR.tile(512) for _ in range(2)]
            ropeT_v = ropeT_d[g].rearrange("i p t -> p i t")
            ro_t = [ro, AR.tile(896)]
            pend = None

            def emit_tr(pd, rs, rs3):
                ro_, t4_ = pd
                pb = bankbf(6)
                for i in range(7):
                    tr(pb[:, i * 128:(i + 1) * 128], ro_.ap[:, i * 128:(i + 1) * 128], ident.ap, [ro_, ident], [banks[6]])
                cp("act", rs3[:, :, t4_ * 128:(t4_ + 1) * 128], r3(pb[:, 0:896], 7), [banks[6]], [rs])
            dma("sp", r3(hbuf[0].ap, 16), hT_v[:, :, 0:512], [dres("hT", 0)], [hbuf[0]])
            for tg in range(8):
                hb = hbuf[tg % 2]
                hb3 = r3(hb.ap, 16)
                if tg + 1 < 8:
                    dma("sp", r3(hbuf[(tg + 1) % 2].ap, 16), hT_v[:, :, (tg + 1) * 512:(tg + 2) * 512], [dres("hT", tg + 1)],
                        [hbuf[(tg + 1) % 2]])
                for ch in range(9):
                    b = 4 + (ch % 2)
                    c0 = 1676 + ch * 128
                    for dc in range(16):
                        mm(banks[b].ap, W3[:, dc, c0:c0 + 128], hb3[:, dc, :], dc == 0, dc == 15, [W, hb], [banks[b]])
                    fs = fstage[ch % 2]
                    sc_ = SCALE if 1 <= ch <= 4 else 1.0
                    S_.add("act", lambda e, o=fs.ap, i=banks[b].ap, s=sc_: e.mul(out=o, in_=i, mul=s), rl([banks[b]]), rl([fs]))
                    dma("sp", featT_d[g, ch, :, tg * 512:(tg + 1) * 512], fs.ap, [fs], [dres("featT", g, ch, tg)])
                rs = rstage[0]
                rs3 = r3(rs.ap, 7)
                for t4 in range(4):
                    tt = tg * 4 + t4
                    for dc in range(16):
                        l_ = hb3[:, dc, t4 * 128:(t4 + 1) * 128]
                        mm(banks[0].ap, l_, W3[:, dc, 0:512], dc == 0, dc == 15, [W, hb], [banks[0]])
                        mm(banks[1].ap[:, 0:384], l_, W3[:, dc, 512:896], dc == 0, dc == 15, [W, hb], [banks[1]])
                        mm(banks[2].ap[:, 0:268], l_, W3[:, dc, 896:1164], dc == 0, dc == 15, [W, hb], [banks[2]])
                        mm(banks[3].ap, l_, W3[:, dc, 1164:1676], dc == 0, dc == 15, [W, hb], [banks[3]])
                    if pend is not None:
                        emit_tr(pend, rs, rs3)
                        pend = None
                    ro = ro_t[tt % 2]
                    vs_ = vstage[tt % 2]
                    cp("dve", vs_.ap, banks[2].ap[:, 0:256], [banks[2]], [vs_])
                    dma("sp", vsw_d[g, tt * 128:(tt + 1) * 128, :], vs_.ap, [vs_], [dres("vsw", g, tt)])
                    gs = gstage[tt % 2]
                    act(gexp.ap, banks[2].ap[:, 256:268], AF.Exp, [banks[2]], [gexp], scale=-1.0)
                    ts_("dve", gexp.ap, gexp.ap, 1.0, None, ALU.add, None, [gexp], [gexp])
                    recip(gs.ap, gexp.ap, [gexp], [gs])
                    dma("sp", gat_d[g, tt * 128:(tt + 1) * 128, :], gs.ap, [gs], [dres("gat", g, tt)])
                    cs = cstage[tt % 2]
                    cp("act", cs.ap, banks[3].ap, [banks[3]], [cs])
                    dma("sp", vsb_d[g, tt * 128:(tt + 1) * 128, :], cs.ap, [cs], [dres("vsb", g, tt)])
                    cp("act", xr.ap[:, 0:512], banks[0].ap, [banks[0]], [xr])
                    cp("dve", xr.ap[:, 512:896], banks[1].ap[:, 0:384], [banks[1], xr], [xr])
                    tt_("dve", sq.ap, xr.ap, xr.ap, ALU.mult, [xr], [sq])
                    rsum(ss7.ap, r3(sq.ap, 7), [sq], [ss7])
                    rstd_lnexp(ss7.ap, ss7.ap, 1.0 / 128, [ss7], [ss7], l7)
                    mset("dve", ss7.ap[:, 4:5], 1.0, [ss7])
                    tt_("dve", r3(sq.ap, 7), r3(xr.ap, 7), ss7.ap.unsqueeze(2).broadcast_to([128, 7, 128]), ALU.mult,
                        [xr, ss7], [sq])
                    tt_("dve", xr.ap, sq.ap, g7.ap, ALU.mult, [sq, g7], [xr])
                    x3 = r3(xr.ap, 7)
                    x1v = x3[:, :, 0:64]
                    x2v = x3[:, :, 64:128]
                    cb = cos3[:, tt, :].unsqueeze(1).broadcast_to([128, 7, 64])
                    sb_ = sin3[:, tt, :].unsqueeze(1).broadcast_to([128, 7, 64])
                    ro3 = r3(ro.ap, 7)
                    tt_("dve", r3(t1.ap, 7), x1v, cb, ALU.mult, [xr, cosT], [t1])
                    tt_("dve", r3(t2.ap, 7), x2v, sb_, ALU.mult, [xr, sinT], [t2])
                    tt_("dve", ro3[:, :, 0:64], r3(t1.ap, 7), r3(t2.ap, 7), ALU.subtract, [t1, t2], [ro])
                    tt_("pool", r3(t3.ap, 7), x2v, cb, ALU.mult, [xr, cosT], [t3])
                    tt_("pool", r3(t4t.ap, 7), x1v, sb_, ALU.mult, [xr, sinT], [t4t])
                    tt_("pool", ro3[:, :, 64:128], r3(t3.ap, 7), r3(t4t.ap, 7), ALU.add, [t3, t4t, ro], [ro])
                    pend = (ro, t4)
                emit_tr(pend, rs, rs3)
                pend = None
                dma("sp", ropeT_v[:, :, tg * 512:(tg + 1) * 512], rs3, [rs], [dres("ropeT", g, tg)])

            phase_reset()
            rope_all = [dres("ropeT", g, tg) for tg in range(8)]
            kcT_s = AR.tile(256)
            vc_aug = [AR.tile(193) for _ in range(2)]
            cmp_mark = AR.off
            kT = AR.tile(S)
            vT = AR.tile(S)
            dma("sp", kT.ap, ropeT_d[g, 4, :, :], rope_all, [kT])
            dma("sp", vT.ap, featT_d[g, 0, :, :], [dres("featT", g, 0, tg) for tg in range(8)], [vT])
            kcn_t = AR.tile(128, F32)
            dma("sp", kcn_t.ap, kcn_in[0:1, :].partition_broadcast(128), [], [kcn_t])
            for nt in range(2):
                dma("pool", vc_aug[nt].ap[:, 128:193], mmat_in[nt * 128:(nt + 1) * 128, :], [], [vc_aug[nt]])
            for kv in range(2):
                src = kT if kv == 0 else vT
                w1 = AR.tile(32 * 256)
                w13 = r3(w1.ap, 32)
                dma("pool", w13, cw1[kv].rearrange("(l d) h -> d l h", d=128), [], [w1])
                w2 = AR.tile(2 * 128)
                w23 = r3(w2.ap, 2)
                dma("pool", w23, cw2[kv].rearrange("(c p) d -> p c d", p=128), [], [w2])
                pst = AR.tile(128)
                dma("pool", pst.ap[0:32, :], cpos[kv][:, :], [], [pst])
                posT = AR.tile(32)
                pb = bankbf(7)
                tr(pb[:, 0:32], pst.ap[0:32, :], ident.ap[0:32, 0:32], [pst, ident], [banks[7]])
                cp("dve", posT.ap, pb[:, 0:32], [banks[7]], [posT])
                cst = AR.tile(2, F32)
                gT = [AR.tile(256) for _ in range(2)]
                u = AR.tile(256, F32)
                u2 = AR.tile(256, F32)
                th = AR.tile(256, F32)
                for hc in range(2):
                    for l in range(32):
                        mm(banks[5].ap[:, hc:hc + 1], w13[:, l, hc * 128:(hc + 1) * 128], posT.ap[:, l:l + 1], l == 0, l == 31,
                           [w1, posT], [banks[5]])
                cp("dve", cst.ap, banks[5].ap[:, 0:2], [banks[5]], [cst])
                for hc in range(2):
                    b = 4
                    for l in range(32):
                        mm(banks[b].ap[:, 0:255], w13[:, l, hc * 128:(hc + 1) * 128], src.ap[:, l:l + 16 * 254 + 1:16],
                           l == 0, l == 31, [w1, src], [banks[b]])
                    act(u.ap[:, 0:255], banks[b].ap[:, 0:255], AF.Identity, [banks[b], cst], [u], bias=cst.ap[:, hc:hc + 1])
                    tt_("dve", u2.ap[:, 0:255], u.ap[:, 0:255], u.ap[:, 0:255], ALU.mult, [u], [u2])
                    ts_("dve", u2.ap[:, 0:255], u2.ap[:, 0:255], 0.044715, 1.0, ALU.mult, ALU.add, [u2], [u2])
                    tt_("dve", u2.ap[:, 0:255], u2.ap[:, 0:255], u.ap[:, 0:255], ALU.mult, [u2, u], [u2])
                    act(th.ap[:, 0:255], u2.ap[:, 0:255], AF.Tanh, [u2], [th], scale=0.7978845608028654)
                    stt("dve", gT[hc].ap[:, 0:255], th.ap[:, 0:255], 1.0, u.ap[:, 0:255], ALU.add, ALU.mult, [th, u], [gT[hc]])
                for nt in range(2):
                    rows = 128 if nt == 0 else 127
                    b = 5
                    for hc in range(2):
                        mm(banks[b].ap[0:rows, 0:128], gT[hc].ap[:, nt * 128:nt * 128 + rows], w23[:, hc, :], hc == 0, hc == 1,
                           [gT[hc], w2], [banks[b]])
                    if kv == 0:
                        kc = AR.tile(128, F32)
                        kq = AR.tile(128, F32)
                        kss = AR.tile(1, F32)
                        kl = AR.tile(1, F32)
                        kb = AR.tile(128)
                        S_.add("act", lambda e, o=kc.ap[0:rows, :], i=banks[b].ap[0:rows, 0:128]: e.mul(out=o, in_=i, mul=0.5),
                               rl([banks[b]]), rl([kc]))
                        tt_("dve", kq.ap[0:rows, :], kc.ap[0:rows, :], kc.ap[0:rows, :], ALU.mult, [kc], [kq])
                        rsum(kss.ap[0:rows, :], kq.ap[0:rows, :], [kq], [kss])
                        rstd_lnexp(kss.ap[0:rows, :], kss.ap[0:rows, :], 1.0 / 128, [kss], [kss], T(kl.ap[0:rows, :], kl.res))
                        stt("dve", kb.ap[0:rows, :], kc.ap[0:rows, :], kss.ap[0:rows, :], kcn_t.ap[0:rows, :], ALU.mult, ALU.mult,
                            [kc, kss, kcn_t], [kb])
                        pb = bankbf(7)
                        tr(pb[:, 0:rows], kb.ap[0:rows, :], ident.ap[0:rows, 0:rows], [kb, ident], [banks[7]])
                        cp("dve", kcT_s.ap[:, nt * 128:nt * 128 + rows], pb[:, 0:rows], [banks[7]], [kcT_s])
                    else:
                        S_.add("act", lambda e, o=vc_aug[nt].ap[0:rows, 0:128], i=banks[b].ap[0:rows, 0:128]: e.mul(out=o, in_=i, mul=0.5),
                               rl([banks[b]]), rl([vc_aug[nt]]))
            if DEBUG:
                dma("sp", dbg_kc[g, :, :], kcT_s.ap, [kcT_s], [dres("dbgkc", g)])
                for nt in range(2):
                    dma("sp", dbg_vc[g, nt * 128:(nt + 1) * 128, :], vc_aug[nt].ap, [vc_aug[nt]], [dres("dbgvc", g, nt)])
            S_.barrier()
            AR.off = cmp_mark

            qT = AR.tile(4 * S)
            qT3 = r3(qT.ap, 4)
            dma("sp", qT3, ropeT_d[g, 0:4, :, :].rearrange("i p t -> p i t"), rope_all, [qT])
            ksT = AR.tile(S)
            kwT = AR.tile(S)
            dma("sp", ksT.ap, ropeT_d[g, 5, :, :], rope_all, [ksT])
            dma("sp", kwT.ap, ropeT_d[g, 6, :, :], rope_all, [kwT])
            vsa = AR.tile(NT * 129)
            vwa = AR.tile(NT * 129)
            vsa3 = r3(vsa.ap, NT)
            vwa3 = r3(vwa.ap, NT)
            vsw_all = [dres("vsw", g, tt) for tt in range(NT)]
            vsw_v = vsw_d[g].rearrange("(n p) c -> p n c", p=128)
            dma("sp", vsa3[:, :, 0:128], vsw_v[:, :, 0:128], vsw_all, [vsa])
            dma("sp", vwa3[:, :, 0:128], vsw_v[:, :, 128:256], vsw_all, [vwa])
            mset("dve", vsa3[:, :, 128:129], 1.0, [vsa])
            mset("dve", vwa3[:, :, 128:129], 1.0, [vwa])
            gat = AR.tile(NT * 12, F32)
            gat3 = r3(gat.ap, NT)
            dma("sp", gat3, gat_d[g].rearrange("(n p) c -> p n c", p=128), [dres("gat", g, tt) for tt in range(NT)], [gat])
            cbt = AR.tile(2 * S)
            cbt3 = r3(cbt.ap, 2)
            dma("pool", cbt3, cbt_in.rearrange("(n p) t -> p n t", p=128), [], [cbt])
            atab = AR.tile(NT * 64, F32)
            btab = AR.tile(NT * 64, F32)
            dma("sp", r3(atab.ap, NT), atab_in.rearrange("(n p) c -> p n c", p=128), [], [atab])
            dma("sp", r3(btab.ap, NT), btab_in.rearrange("(n p) c -> p n c", p=128), [], [btab])
            ee = AR.tile(S)
            dma("pool", ee.ap[0:64, :], ee_in[:, :], [], [ee])
            tri4 = AR.tile(512)
            tri24 = AR.tile(512)
            dma("pool", tri4.ap, tri4_in[:, :], [], [tri4])
            dma("pool", tri24.ap, tri24_in[:, :], [], [tri24])
            gon = AR.tile(512, F32)
            dma("sp", gon.ap, nsaon_in[g:g + 1, :].partition_broadcast(128), [], [gon])
            Pc = [AR.tile(512) for _ in range(2)]
            Ps = [AR.tile(512) for _ in range(4)]
            msk_t = [AR.tile(128) for _ in range(4)]
            mres = [Res() for _ in range(4)]
            SBK = [0, 1, 6]
            oc = AR.tile(4 * 193, F32)
            oc3 = r3(oc.ap, 4)
            osl = AR.tile(4 * 129, F32)
            osl3 = r3(osl.ap, 4)
            owi = AR.tile(4 * 129, F32)
            owi3 = r3(owi.ap, 4)
            rd = AR.tile(12, F32)
            rd3 = r3(rd.ap, 3)
            cf = AR.tile(12, F32)
            cf3 = r3(cf.ap, 3)
            psl = AR.tile(64, F32)
            sc = AR.tile(64, F32)
            sc2 = AR.tile(64, F32)
            mx = AR.tile(16, F32)
            selb = AR.tile(64)
            selT4 = AR.tile(512)
            o1 = AR.tile(512, F32)
            o2 = AR.tile(512, F32)
            o3 = AR.tile(512, F32)
            ssq = AR.tile(4, F32)
            l4 = AR.tile(4, F32)
            yb = AR.tile(512)
            ystage = [AR.tile(4 * 512) for _ in range(2)]
            yT_v = yT_d.rearrange("c p t -> p c t")
            PSI = [0]
            oc_t = [oc, AR.tile(4 * 193, F32)]
            rdc_t = [AR.tile(4, F32) for _ in range(2)]
            selT4_t = [selT4, AR.tile(512)]

            CHAIN = []
            X2 = [None]

            def stage_x(tt):
                Q = qT3[:, :, tt * 128:(tt + 1) * 128]
                oc = oc_t[tt % 2]
                oc3 = r3(oc.ap, 4)
                rdc = rdc_t[tt % 2]
                selT4 = selT4_t[tt % 2]
                nts = [0] if tt < 16 else [0, 1]
                for nt in nts:
                    rows = 128 if nt == 0 else 127
                    needm = (nt == 1) or (tt <= 16)
                    mm(r3(banks[nt].ap[0:rows, :], 4), kcT_s.ap[:, nt * 128:nt * 128 + rows], Q, True, not needm,
                       [kcT_s, qT], [banks[nt]])
                    if needm:
                        for r in range(4):
                            mm(banks[nt].ap[0:rows, r * 128:(r + 1) * 128], ident.ap[0:rows, 0:rows],
                               cbt3[0:rows, nt, tt * 128:(tt + 1) * 128], False, r == 3, [ident, cbt], [banks[nt]])
                    act(Pc[nt].ap[0:rows, :], banks[nt].ap[0:rows, :], AF.Exp, [banks[nt]], [Pc[nt]], scale=SCALE)
                for r in range(4):
                    b = 2 + r
                    for i, nt in enumerate(nts):
                        rows = 128 if nt == 0 else 127
                        mm(banks[b].ap[:, 0:193], Pc[nt].ap[0:rows, r * 128:(r + 1) * 128],
                           vc_aug[nt].ap[0:rows, :], i == 0, i == len(nts) - 1, [Pc[nt], vc_aug[nt]], [banks[b]])
                for r in range(4):
                    cp("act" if r % 2 == 0 else "dve", oc3[:, r, :], banks[2 + r].ap[:, 0:193], [banks[2 + r], oc], [oc])
                ch = CHAIN
                ch.append(lambda: ts_("dve", rdc.ap, oc3[:, :, 192], 1e-30, None, ALU.max, None, [oc], [rdc]))
                ch.append(lambda: recip(rdc.ap, rdc.ap, [rdc], [rdc]))
                ch.append(lambda: ts_("dve", psl.ap, oc3[:, 0, 128:192], rdc.ap[:, 0:1], None, ALU.mult, None, [oc, rdc], [psl]))
                for r in range(1, 4):
                    ch.append(lambda r=r: stt("dve", psl.ap, oc3[:, r, 128:192], rdc.ap[:, r:r + 1], psl.ap, ALU.mult, ALU.add,
                                              [oc, rdc, psl], [psl]))
                ch.append(lambda: tt_("dve", sc.ap, psl.ap, r3(atab.ap, NT)[:, tt, :], ALU.mult, [psl, atab], [sc]))
                ch.append(lambda: tt_("dve", sc.ap, sc.ap, r3(btab.ap, NT)[:, tt, :], ALU.add, [sc, btab], [sc]))
                ch.append(lambda: S_.add("dve", lambda e: e.max(out=mx.ap[:, 0:8], in_=sc.ap), rl([sc]), rl([mx])))
                ch.append(lambda: S_.add("dve", lambda e: e.match_replace(out=sc2.ap, in_to_replace=mx.ap[:, 0:8], in_values=sc.ap,
                                                                          imm_value=-1e30), rl([sc, mx]), rl([sc2])))
                ch.append(lambda: S_.add("dve", lambda e: e.max(out=mx.ap[:, 8:16], in_=sc2.ap), rl([sc2, mx]), rl([mx])))
                ch.append(lambda: ts_("dve", sc2.ap, sc.ap, mx.ap[:, 15:16], None, ALU.is_ge, None, [sc, mx, sc2], [sc2]))
                ch.append(lambda: cp("dve", selb.ap, sc2.ap, [sc2], [selb]))

                def x2(selT4=selT4):
                    pb = bankbf(6)
                    tr(pb[0:64, 0:128], selb.ap, ident.ap, [selb, ident], [banks[6]])
                    cp("dve", selT4.ap[0:64, 0:128], pb[0:64, 0:128], [banks[6], selT4], [selT4])
                X2[0] = x2

            def drain_chain(n=None):
                k = 0
                while CHAIN and (n is None or k < n):
                    CHAIN.pop(0)()
                    k += 1

            def stage_y(tt):
                Q = qT3[:, :, tt * 128:(tt + 1) * 128]
                oc = oc_t[tt % 2]
                oc3 = r3(oc.ap, 4)
                rdc = rdc_t[tt % 2]
                selT4 = selT4_t[tt % 2]
                k0 = max(0, tt - 4)
                its = [("s", kt) for kt in range(tt + 1)] + [("w", kt) for kt in range(k0, tt + 1)]

                def emit_s(j, tt=tt, Q=Q, its=its, k0=k0):
                    kind, kt = its[j]
                    q = psi0 + j
                    Sb = banks[SBK[q % 3]]
                    P_ = Ps[q % 4]
                    diag = kt == tt
                    if kind == "s":
                        mm(r3(Sb.ap, 4), ksT.ap[:, kt * 128:(kt + 1) * 128], Q, True, not diag, [ksT, qT], [Sb])
                        if diag:
                            mm(Sb.ap, ident.ap, tri4.ap, False, True, [ident, tri4], [Sb])
                        mreg = banks[7].ap[:, (q % 4) * 128:(q % 4) * 128 + 128]
                        mm(mreg, ee.ap[0:64, kt * 128:(kt + 1) * 128], selT4.ap[0:64, 0:128], True, True, [ee, selT4], [banks[7]])
                        act(P_.ap, Sb.ap, AF.Exp, [Sb], [P_], scale=SCALE)
                        mk = msk_t[q % 4]
                        cp("act", mk.ap, mreg, [banks[7]], [mk])
                        tt_("dve", r3(P_.ap, 4), r3(P_.ap, 4), mk.ap.unsqueeze(1).broadcast_to([128, 4, 128]), ALU.mult,
                            [P_, mk], [P_])
                    else:
                        low = kt == tt - 4
                        mm(r3(Sb.ap, 4), kwT.ap[:, kt * 128:(kt + 1) * 128], Q, True, not (diag or low), [kwT, qT], [Sb])
                        if diag:
                            mm(Sb.ap, ident.ap, tri4.ap, False, True, [ident, tri4], [Sb])
                        if low:
                            mm(Sb.ap, ident.ap, tri24.ap, False, True, [ident, tri24], [Sb])
                        act(P_.ap, Sb.ap, AF.Exp, [Sb], [P_], scale=SCALE)

                def emit_av(j, tt=tt, its=its, k0=k0):
                    kind, kt = its[j]
                    P_ = Ps[(psi0 + j) % 4]
                    for r in range(4):
                        bo = 2 + r
                        if kind == "s":
                            mm(banks[bo].ap[:, 0:129], P_.ap[:, r * 128:(r + 1) * 128], vsa3[:, kt, :],
                               kt == 0, kt == tt, [P_, vsa], [banks[bo]])
                        else:
                            mm(banks[bo].ap[:, 0:129], P_.ap[:, r * 128:(r + 1) * 128], vwa3[:, kt, :],
                               kt == k0, kt == tt, [P_, vwa], [banks[bo]])
                    if kind == "s" and kt == tt:
                        for r in range(4):
                            cp("act" if r % 2 == 0 else "dve", osl3[:, r, :], banks[2 + r].ap[:, 0:129], [banks[2 + r], osl], [osl])

                psi0 = PSI[0]
                nn = len(its)
                for j in range(min(2, nn)):
                    emit_s(j)
                for j in range(nn):
                    if j + 2 < nn:
                        emit_s(j + 2)
                    emit_av(j)
                    drain_chain(1)
                PSI[0] += nn
                drain_chain()
                if X2[0] is not None:
                    X2[0]()
                    X2[0] = None
                cp("dve", rd3[:, 0, :], rdc.ap, [rdc, rd], [rd])
                for r in range(4):
                    cp("act" if r % 2 == 0 else "dve", owi3[:, r, :], banks[2 + r].ap[:, 0:129], [banks[2 + r], owi], [owi])
                recip(rd3[:, 1, :], osl3[:, :, 128], [osl, rd], [rd])
                recip(rd3[:, 2, :], owi3[:, :, 128], [owi, rd], [rd])
                tt_("dve", cf3, rd3, r3(gat3[:, tt, :], 4).rearrange("p h c -> p c h"), ALU.mult, [rd, gat], [cf])
                bc = lambda i: cf3[:, i, :].unsqueeze(2).broadcast_to([128, 4, 128])
                tt_("dve", r3(o1.ap, 4), oc3[:, :, 0:128], bc(0), ALU.mult, [oc, cf], [o1])
                tt_("pool", r3(o2.ap, 4), osl3[:, :, 0:128], bc(1), ALU.mult, [osl, cf], [o2])
                tt_("dve", r3(o3.ap, 4), owi3[:, :, 0:128], bc(2), ALU.mult, [owi, cf], [o3])
                tt_("pool", o1.ap, o1.ap, o2.ap, ALU.add, [o1, o2], [o1])
                tt_("dve", o1.ap, o1.ap, o3.ap, ALU.add, [o1, o3], [o1])
                tt_("dve", o2.ap, o1.ap, o1.ap, ALU.mult, [o1, o2], [o2])
                rsum(ssq.ap, r3(o2.ap, 4), [o2], [ssq])
                rstd_lnexp(ssq.ap, ssq.ap, 1.0 / 128, [ssq], [ssq], l4)
                tt_("dve", r3(o2.ap, 4), r3(o1.ap, 4), ssq.ap.unsqueeze(2).broadcast_to([128, 4, 128]), ALU.mult, [o1, ssq], [o2])
                tt_("dve", yb.ap, o2.ap, gon.ap, ALU.mult, [o2, gon], [yb])
                pb = bankbf(6)
                for r in range(4):
                    tr(pb[:, r * 128:(r + 1) * 128], yb.ap[:, r * 128:(r + 1) * 128], ident.ap, [yb, ident], [banks[6]])
                ys = ystage[(tt // 4) % 2]
                ys3 = r3(ys.ap, 4)
                cp("act", ys3[:, :, (tt % 4) * 128:(tt % 4) * 128 + 128], r3(pb[:, 0:512], 4), [banks[6]], [ys])
                if tt % 4 == 3:
                    tg = tt // 4
                    dma("sp", yT_v[:, 4 * g:4 * g + 4, tg * 512:(tg + 1) * 512], ys3, [ys], [dres("yT", g, tg)])


            stage_x(0)
            drain_chain()
            X2[0]()
            X2[0] = None
            for tt in range(NT):
                if tt + 1 < NT:
                    stage_x(tt + 1)
                stage_y(tt)

            phase_reset()
            qs = AR.tile(4 * S)
            ks_ = AR.tile(4 * S)
            qs3 = r3(qs.ap, 4)
            ks3 = r3(ks_.ap, 4)
            fall = lambda c0, c1: [dres("featT", g, ch, tg) for ch in range(c0, c1) for tg in range(8)]
            dma("sp", qs3, featT_d[g, 1:5, :, :].rearrange("i p t -> p i t"), fall(1, 5), [qs])
            dma("sp", ks3, featT_d[g, 5:9, :, :].rearrange("i p t -> p i t"), fall(5, 9), [ks_])
            vsbt = AR.tile(NT * 512)
            vsb3 = r3(vsbt.ap, NT)
            dma("sp", vsb3, vsb_d[g].rearrange("(n p) c -> p n c", p=128), [dres("vsb", g, tt) for tt in range(NT)], [vsbt])
            sbm = AR.tile(2048)
            sbm3 = r3(sbm.ap, 4)
            dma("pool", sbm.ap, sbm_in[:, :], [], [sbm])
            ust = AR.tile(128)
            ule = AR.tile(128)
            onesb = AR.tile(128)
            dma("pool", ust.ap, ust_in[:, :], [], [ust])
            dma("pool", ule.ap, ule_in[:, :], [], [ule])
            mset("dve", onesb.ap, 1.0, [onesb])
            sbon = AR.tile(8, F32)
            dma("sp", sbon.ap, sbon_in[:, :], [], [sbon])
            LA = 4
            NB = 6
            e_t = [AR.tile(512, F32) for _ in range(2)]
            sp_t = [AR.tile(512, F32) for _ in range(2)]
            spb_t = [AR.tile(512) for _ in range(NB)]
            a1_t = [AR.tile(512, F32) for _ in range(NB)]
            at_t = [AR.tile(512) for _ in range(NB)]
            sqb = AR.tile(512)
            od = AR.tile(512, F32)
            rst = AR.tile(512, F32)
            lnt = AR.tile(512, F32)
            yst = [AR.tile(512) for _ in range(2)]
            items = []
            for h in range(4):
                for qc in range(8):
                    kts = list(range(4 * qc + 3, -1, -1))
                    for ki_, kt in enumerate(kts):
                        items.append((h, qc, kt, ki_ == 0, ki_ == len(kts) - 1, (h * 8 + qc) % 2))
            n_it = len(items)

            def a_pe(i):
                h, qc, kt, first, last, sidx = items[i]
                zb = banks[i % 2]
                o = kt - 4 * qc
                mm(zb.ap, ks3[:, h, kt * 128:(kt + 1) * 128], qs3[:, h, qc * 512:(qc + 1) * 512], True, o < 0,
                   [ks_, qs], [zb])
                if o >= 0:
                    mm(zb.ap, ident.ap, sbm3[:, o, :], False, True, [ident, sbm], [zb])

            def a_act(i):
                zb = banks[i % 2]
                e_ = e_t[i % 2]
                sp_ = sp_t[i % 2]
                act(e_.ap, zb.ap, AF.Exp, [zb], [e_])
                act(sp_.ap, e_.ap, AF.Ln, [e_, one_t], [sp_], bias=one_t.ap)

            def a_post(i):
                zb = banks[i % 2]
                sp_ = sp_t[i % 2]
                spb = spb_t[i % NB]
                a1 = a1_t[i % NB]
                tt_("dve", a1.ap, zb.ap, sp_.ap, ALU.subtract, [zb, sp_], [a1])
                cp("pool", spb.ap, sp_.ap, [sp_], [spb])

            def b1_pe(i):
                h, qc, kt, first, last, sidx = items[i]
                Cb = banks[2 + sidx]
                mm(Cb.ap, ust.ap, spb_t[i % NB].ap, first, True, [ust, spb_t[i % NB]], [Cb])

            def b1_dve(i):
                h, qc, kt, first, last, sidx = items[i]
                Cb = banks[2 + sidx]
                a1 = a1_t[i % NB]
                tt_("dve", a1.ap, a1.ap, Cb.ap, ALU.subtract, [a1, Cb], [a1])

            def b1_act(i):
                a1 = a1_t[i % NB]
                at = at_t[i % NB]
                act(at.ap, a1.ap, AF.Exp, [a1], [at])

            def stage_b2(i):
                h, qc, kt, first, last, sidx = items[i]
                Cb = banks[2 + sidx]
                spb = spb_t[i % NB]
                mm(Cb.ap, ule.ap, spb.ap, False, True, [ule, spb], [Cb])

            def stage_av(i):
                h, qc, kt, first, last, sidx = items[i]
                Db = banks[4 + sidx]
                at = at_t[i % NB]
                mm(Db.ap, vsb3[:, kt, h * 128:(h + 1) * 128], at.ap, first, last, [vsbt, at], [Db])
                if last:
                    cp("act", od.ap, Db.ap, [Db], [od])
                    tt_("dve", sqb.ap, od.ap, od.ap, ALU.mult, [od], [sqb])
                    mm(banks[6].ap, onesb.ap, sqb.ap, True, True, [onesb, sqb], [banks[6]])
                    act(lnt.ap, banks[6].ap, AF.Ln, [banks[6]], [lnt], bias=eps_t.ap, scale=1.0 / 128)
                    act(rst.ap, lnt.ap, AF.Exp, [lnt], [rst], scale=-0.5)
                    ys = yst[(h * 8 + qc) % 2]
                    stt("dve", ys.ap, od.ap, sbon.ap[:, 4 * g + h:4 * g + h + 1], rst.ap, ALU.mult, ALU.mult, [od, sbon, rst], [ys])
                    dma("sp", yT_d[8 + 4 * g + h, :, qc * 512:(qc + 1) * 512], ys.ap, [ys], [dres("yTsb", g, h, qc)])

            for i in range(LA):
                a_pe(i)
                a_act(i)
                a_post(i)
            for i in range(n_it + 1):
                if i < n_it:
                    b1_pe(i)
                    b1_dve(i)
                if i + LA < n_it:
                    a_pe(i + LA)
                    a_act(i + LA)
                    a_post(i + LA)
                if i < n_it:
                    b1_act(i)
                if 1 <= i:
                    stage_av(i - 1)
                if i < n_it:
                    stage_b2(i)

        phase_reset()
        wo = AR.tile(16 * D)
        wo3 = r3(wo.ap, 16)
        wov = w_out.rearrange("(c p) f -> p c f", p=128)
        for q in range(4):
            dma("pool", wo3[:, q * 4:(q + 1) * 4, :], wov[:, q * 4:(q + 1) * 4, :], [wo] if q else [], [wo])
        yall = [dres("yT", g, tg) for g in range(2) for tg in range(8)] + \
               [dres("yTsb", g, h, qc) for g in range(2) for h in range(4) for qc in range(8)]
        ybA = [AR.tile(16 * 512) for _ in range(2)]
        ybB = AR.tile(16 * 512)
        xa = [AR.tile(D, F32) for _ in range(2)]
        xb_ = AR.tile(D, F32)
        x2t = [AR.tile(D, F32) for _ in range(2)]
        yT_v = yT_d.rearrange("c p t -> p c t")
        for tg in range(4):
            ya = ybA[tg % 2]
            ya3 = r3(ya.ap, 16)
            dma("sp", ya3, yT_v[:, :, tg * 512:(tg + 1) * 512], yall, [ya])
            dma("sp", r3(ybB.ap, 16), yT_v[:, :, 2048 + tg * 512:2048 + (tg + 1) * 512], yall, [ybB])
            ts_("dve", ya.ap, ya.ap, sel_t.ap[:, 0:1], None, ALU.mult, None, [ya, sel_t], [ya])
            stt("dve", ya.ap, ybB.ap, sel_t.ap[:, 1:2], ya.ap, ALU.mult, ALU.add, [ybB, sel_t, ya], [ya])
            for t4 in range(4):
                tl = tg * 4 + t4
                xt = xa[tl % 2]
                dma("sp", xt.ap, x1_d[tl * 128:(tl + 1) * 128, :], [dres("x1", tl, q) for q in range(4)], [xt])
                dma("sp", xb_.ap, x1_d[2048 + tl * 128:2048 + (tl + 1) * 128, :], [dres("x1", 16 + tl, q) for q in range(4)], [xb_])
                ts_("dve", xt.ap, xt.ap, sel_t.ap[:, 0:1], None, ALU.mult, None, [xt, sel_t], [xt])
                stt("dve", xt.ap, xb_.ap, sel_t.ap[:, 1:2], xt.ap, ALU.mult, ALU.add, [xb_, sel_t, xt], [xt])
                x2 = x2t[tl % 2]
                for dmc in range(4):
                    b = (tl * 4 + dmc) % 8
                    for c in range(16):
                        mm(banks[b].ap, ya3[:, c, t4 * 128:(t4 + 1) * 128], wo3[:, c, dmc * 512:(dmc + 1) * 512], c == 0, c == 15,
                           [ya, wo], [banks[b]])
                    tt_("dve", x2.ap[:, dmc * 512:(dmc + 1) * 512], banks[b].ap, xt.ap[:, dmc * 512:(dmc + 1) * 512], ALU.add,
                        [banks[b], xt], [x2])
                dma("sp", x2_d[tl * 128:(tl + 1) * 128, :], x2.ap, [x2], [dres("x2", tl)])

        ffn_phase("f2", x2_d, lambda tt: [dres("x2", tt)], 2048, ffw[2], out_d, lambda tt, dmc: dres("out", tt, dmc))

        S_.emit(nc, sems, dsems)
    return nc


def _consts():
    c = {}
    c["ident"] = np.eye(128, dtype=np.float32)
    c["invf"] = (10000.0 ** (-np.arange(0, 128, 2, dtype=np.float32) / 128)).astype(np.float32).reshape(1, 64)
    k = np.arange(128)[:, None]
    t = np.arange(128)[None, :]
    tri = np.where(k > t, NEG, 0.0).astype(np.float32)
    c["tri4"] = np.tile(tri, (1, 4))
    tri2 = np.where(k <= t, NEG, 0.0).astype(np.float32)
    c["tri24"] = np.tile(tri2, (1, 4))
    strict = np.where(k >= t, NEG, 0.0).astype(np.float32)
    sbm = np.zeros((128, 4, 4, 128), np.float32)
    for o in range(4):
        for j in range(4):
            if j < o:
                sbm[:, o, j, :] = NEG
            elif j == o:
                sbm[:, o, j, :] = strict
    c["sbm"] = sbm.reshape(128, 2048)
    n = np.arange(256)[:, None]
    tt = np.arange(S)[None, :]
    c["cbt"] = np.where((16 * n + 31 > tt) | (n >= 255), NEG, 0.0).astype(np.float32)
    mm_ = np.zeros((256, 65), np.float32)
    for blk in range(64):
        for nn in range(4 * blk - 1, 4 * blk + 4):
            if 0 <= nn < 255:
                mm_[nn, blk] = 1.0
    mm_[:255, 64] = 1.0
    c["mmat"] = mm_
    tq = np.arange(S)[:, None]
    blk = np.arange(64)[None, :]
    cur = tq // 64
    forced = (blk == 0) | (blk == cur) | (blk == cur - 1)
    causal = blk * 64 <= tq
    c["atab"] = np.where(causal & ~forced, 1.0, 0.0).astype(np.float32)
    c["btab"] = np.where(causal, np.where(forced, 1e9, 0.0), -1e30).astype(np.float32)
    key = np.arange(S)[None, :]
    c["ee"] = np.where(key // 64 == np.arange(64)[:, None], 1.0, 0.0).astype(np.float32)
    j = np.arange(128)[:, None]
    s = np.arange(128)[None, :]
    c["ust"] = (j > s).astype(np.float32)
    c["ule"] = (j <= s).astype(np.float32)
    return c


def _w_in_groups(w_in):
    outs = []
    for g in range(2):
        cols = []
        cols += list(range(g * 512, g * 512 + 512))
        cols += list(range(1024 + g * 128, 1024 + g * 128 + 128))
        cols += list(range(1536 + g * 128, 1536 + g * 128 + 128))
        cols += list(range(2048 + g * 128, 2048 + g * 128 + 128))
        cols += list(range(1792 + g * 128, 1792 + g * 128 + 128))
        cols += list(range(2304 + g * 128, 2304 + g * 128 + 128))
        cols += list(range(2560 + g * 12, 2560 + g * 12 + 12))
        cols += list(range(4632 + g * 512, 4632 + g * 512 + 512))
        cols += list(range(1280 + g * 128, 1280 + g * 128 + 128))
        cols += list(range(2584 + g * 512, 2584 + g * 512 + 512))
        cols += list(range(3608 + g * 512, 3608 + g * 512 + 512))
        assert len(cols) == 2828
        outs.append(w_in[:, cols])
    return np.ascontiguousarray(np.stack(outs, 0))


_NC_CACHE = {}


def make_in_maps(inputs):
    f = lambda a: np.ascontiguousarray(np.asarray(a, dtype=np.float32))
    x = f(inputs["x"])
    pos = np.asarray(inputs["positions"]).astype(np.int32)
    shared = dict(_consts())
    for i in (1, 2):
        shared[f"f{i}_wg"] = f(inputs[f"ffn{i}_w_gate"][0])
        shared[f"f{i}_wu"] = f(inputs[f"ffn{i}_w_up"][0])
        shared[f"f{i}_wd"] = f(inputs[f"ffn{i}_w_down"][0])
        shared[f"f{i}_norm"] = f(inputs[f"ffn{i}_norm"]).reshape(1, D)
    shared["mix_norm"] = f(inputs["mix_norm"]).reshape(1, D)
    shared["w_in_g"] = _w_in_groups(f(inputs["w_in"][0]))
    qn = f(inputs["nsa_q_norm"]).reshape(128)
    shared["g7"] = np.concatenate([qn, qn, qn, qn, np.ones(128, np.float32), f(inputs["nsa_k_slc_norm"]).reshape(128),
                                   f(inputs["nsa_k_win_norm"]).reshape(128)]).reshape(1, 896)
    shared["kcn"] = f(inputs["nsa_k_cmp_norm"]).reshape(1, 128)
    shared["nsaon"] = f(inputs["nsa_out_norm"]).reshape(2, 512)
    shared["sbon"] = np.ascontiguousarray(f(inputs["sb_out_norm"]).reshape(8, 128).T)
    shared["ck_w1"] = f(inputs["cmp_k_w1"][0])
    shared["cv_w1"] = f(inputs["cmp_v_w1"][0])
    shared["ck_w2"] = f(inputs["cmp_k_w2"][0])
    shared["cv_w2"] = f(inputs["cmp_v_w2"][0])
    shared["ck_pos"] = f(inputs["cmp_k_pos"][0])
    shared["cv_pos"] = f(inputs["cmp_v_pos"][0])
    shared["w_out"] = f(inputs["w_out"][0])
    in_maps = []
    for c in range(8):
        b, r = c // 2, c % 2
        m = dict(shared)
        m["x_b"] = np.ascontiguousarray(x[b])
        m["pos"] = np.ascontiguousarray(pos[b].reshape(NT, 128).T)
        sel = np.zeros((128, 2), np.float32)
        sel[:, r] = 1.0
        m["sel"] = sel
        in_maps.append(m)
    return in_maps


def kernel(**inputs):
    in_maps = make_in_maps(inputs)
    if "nc" not in _NC_CACHE:
        _NC_CACHE["nc"] = build_program()
    nc = _NC_CACHE["nc"]
    res = run_bass_kernel_spmd(nc, in_maps, core_ids=list(range(8)))
    out = np.zeros((4, S, D), np.float32)
    for c in range(8):
        b, r = c // 2, c % 2
        out[b, r * 2048:(r + 1) * 2048, :] = res.results[c]["out"]
    _NC_CACHE["last"] = res
    return out
```
